# Optimizing a Trainium2 kernel written in Bass

```python
import jax, jax.numpy as jnp
from jax import lax
import numpy as np

D_MODEL = 1024
BATCH = 8
SEQ = 2048
DEPTH = 2

GRID_W = 64
CTX_LEN = 256
N_MIXERS = 2
N_MOD = 6
NORM_EPS = 1e-6
CHUNK = 128
GM_WIDTH = 2 * D_MODEL
GM_GROUPS = 8
HEAD_DIM = 128
N_Q_HEADS = D_MODEL // HEAD_DIM
N_KV_HEADS = 2
GQA_GROUP = N_Q_HEADS // N_KV_HEADS
AXIS_DIM = HEAD_DIM // 2
ROPE_THETA = 10000.0
Q_BLOCK = 128
N_EXPERTS = 32
TOP_K = 4
D_EXPERT = D_MODEL
SWIGLU_LIMIT = 7.0
SWIGLU_ALPHA = 1.702
EXPERT_BLOCK = 128
N_A_LAYERS = (DEPTH + 1) // 2
N_B_LAYERS = DEPTH // 2

kernel_name = 'hybrid_gmlp_gqa_moe_diffusion_trunk'


def rmsnorm(x, gain):
    xf = x.astype(jnp.float32)
    y = xf * lax.rsqrt(jnp.mean(xf * xf, axis=-1, keepdims=True) + NORM_EPS)
    return (y * gain.astype(jnp.float32)).astype(x.dtype)


def modulate(h, shift, scale):
    return h * (1 + scale) + shift


def axial_rope_tables(rows):
    row = jnp.repeat(jnp.arange(rows, dtype=jnp.int32), GRID_W).astype(jnp.float32)
    col = jnp.tile(jnp.arange(GRID_W, dtype=jnp.int32), rows).astype(jnp.float32)
    inv_freq = 1.0 / (ROPE_THETA ** (jnp.arange(0, AXIS_DIM, 2, dtype=jnp.float32) / AXIS_DIM))
    ang = jnp.stack([row[:, None] * inv_freq, col[:, None] * inv_freq], axis=0)
    return jnp.cos(ang), jnp.sin(ang)


def apply_axial_rope(x, cos, sin):
    xr = x.reshape(*x.shape[:-1], 2, 2, AXIS_DIM // 2)
    x1, x2 = xr[..., 0, :], xr[..., 1, :]
    cs = jnp.transpose(cos, (1, 0, 2))[None, :, None].astype(x.dtype)
    sn = jnp.transpose(sin, (1, 0, 2))[None, :, None].astype(x.dtype)
    out = jnp.stack([x1 * cs - x2 * sn, x2 * cs + x1 * sn], axis=-2)
    return out.reshape(x.shape)


def chunk_gmlp(h, w_in, b_in, v_gain, w_s, b_s, w_out):
    B_, L, _ = h.shape
    uv = jax.nn.gelu(h @ w_in + b_in, approximate=False)
    u, v = jnp.split(uv, 2, axis=-1)
    v = rmsnorm(v, v_gain)
    v = v.reshape(B_, L // CHUNK, CHUNK, GM_GROUPS, GM_WIDTH // GM_GROUPS)
    s = jnp.einsum('gpq,bnqgc->bnpgc', w_s, v) + b_s.T[:, :, None]
    return (u * s.reshape(B_, L, GM_WIDTH)) @ w_out


def qkv_heads(h, w_qkv, q_gain, k_gain):
    B_, L, _ = h.shape
    qkv = h @ w_qkv
    q, k, v = jnp.split(qkv, [N_Q_HEADS * HEAD_DIM, (N_Q_HEADS + N_KV_HEADS) * HEAD_DIM], axis=-1)
    q = rmsnorm(q.reshape(B_, L, N_Q_HEADS, HEAD_DIM), q_gain)
    k = rmsnorm(k.reshape(B_, L, N_KV_HEADS, HEAD_DIM), k_gain)
    v = v.reshape(B_, L, N_KV_HEADS, HEAD_DIM)
    return q, k, v


def attend(q, k, v):
    B_, Lq = q.shape[:2]
    qg = q.reshape(B_, Lq, N_KV_HEADS, GQA_GROUP, HEAD_DIM)
    s = jnp.einsum('bqkgd,bskd->bkgqs', qg, k).astype(jnp.float32) * (HEAD_DIM ** -0.5)
    p = jax.nn.softmax(s, axis=-1).astype(v.dtype)
    o = jnp.einsum('bkgqs,bskd->bqkgd', p, v)
    return o.reshape(B_, Lq, N_Q_HEADS * HEAD_DIM)


def gqa_mixer(h_lat, h_ctx, w_qkv, q_gain, k_gain, w_o, cos, sin, with_ctx_out):
    B_, L, _ = h_lat.shape
    q_l, k_l, v_l = qkv_heads(h_lat, w_qkv, q_gain, k_gain)
    q_l = apply_axial_rope(q_l, cos, sin)
    k_l = apply_axial_rope(k_l, cos, sin)
    q_c, k_c, v_c = qkv_heads(h_ctx, w_qkv, q_gain, k_gain)
    k_all = jnp.concatenate([k_c, k_l], axis=1)
    v_all = jnp.concatenate([v_c, v_l], axis=1)
    nb = L // Q_BLOCK
    q_blocks = jnp.moveaxis(q_l.reshape(B_, nb, Q_BLOCK, N_Q_HEADS, HEAD_DIM), 1, 0)
    o = lax.map(lambda qb: attend(qb, k_all, v_all), q_blocks)
    o_lat = jnp.moveaxis(o, 0, 1).reshape(B_, L, N_Q_HEADS * HEAD_DIM) @ w_o
    if with_ctx_out:
        o_ctx = attend(q_c, k_c, v_c) @ w_o
        return o_lat, o_ctx
    return o_lat, None


def moe_ffn(h, router_w, router_b, w_gu, b_gu, w_down, b_down):
    T, D = h.shape
    logits = (h @ router_w + router_b).astype(jnp.float32)
    top_val, top_idx = lax.top_k(logits, TOP_K)
    gates = jax.nn.softmax(top_val, axis=-1)
    n_assign = T * TOP_K
    flat_e = top_idx.reshape(-1)
    order = jnp.argsort(flat_e, stable=True)
    sorted_e = flat_e[order]
    tok = (order // TOP_K).astype(jnp.int32)
    gate_sorted = gates.reshape(-1)[order]
    counts = jnp.bincount(flat_e, length=N_EXPERTS)
    padded = (counts + EXPERT_BLOCK - 1) // EXPERT_BLOCK * EXPERT_BLOCK
    pad_end = jnp.cumsum(padded)
    pad_start = pad_end - padded
    grp_start = jnp.cumsum(counts) - counts
    dest = pad_start[sorted_e] + jnp.arange(n_assign, dtype=jnp.int32) - grp_start[sorted_e]
    n_blocks = -(-n_assign // EXPERT_BLOCK) + N_EXPERTS
    cap = n_blocks * EXPERT_BLOCK
    buf_tok = jnp.full((cap,), T, jnp.int32).at[dest].set(tok)
    buf_gate = jnp.zeros((cap,), jnp.float32).at[dest].set(gate_sorted)
    block_expert = jnp.minimum(
        jnp.searchsorted(pad_end, jnp.arange(n_blocks, dtype=jnp.int32) * EXPERT_BLOCK, side='right'),
        N_EXPERTS - 1)
    h_pad = jnp.concatenate([h, jnp.zeros((1, D), h.dtype)], axis=0)
    xb = h_pad[buf_tok].reshape(n_blocks, EXPERT_BLOCK, D)

    def expert_block(args):
        xblk, e = args
        gu = xblk @ w_gu[e] + b_gu[e]
        g, u = jnp.split(gu, 2, axis=-1)
        g = jnp.minimum(g, SWIGLU_LIMIT)
        u = jnp.clip(u, -SWIGLU_LIMIT, SWIGLU_LIMIT)
        act = g * jax.nn.sigmoid(SWIGLU_ALPHA * g) * (u + 1)
        return act @ w_down[e] + b_down[e]

    yb = lax.map(expert_block, (xb, block_expert)).reshape(cap, D)
    y = jax.ops.segment_sum(yb * buf_gate[:, None].astype(yb.dtype), buf_tok, num_segments=T + 1)
    return y[:T]


def setup_inputs(seed: int = 0) -> dict:
    key = jax.random.key(seed)
    ks = iter(jax.random.split(key, 32))
    f32 = jnp.float32

    def nrm(shape, scale):
        return jax.random.normal(next(ks), shape, f32) * scale

    def gain(shape):
        return jnp.ones(shape, f32) + nrm(shape, 0.05)

    D = D_MODEL
    qkv_w = (N_Q_HEADS + 2 * N_KV_HEADS) * HEAD_DIM
    return {
        'x': nrm((BATCH, SEQ, D), 1.0),
        'c': nrm((BATCH, D), 1.0),
        'ctx': nrm((BATCH, CTX_LEN, D), 1.0),
        'c_ctx': nrm((D,), 1.0),
        'ada_w': nrm((DEPTH, D, N_MOD * D), 0.5 * D ** -0.5),
        'ada_b': nrm((DEPTH, N_MOD * D), 0.02),
        'norm_mix': gain((DEPTH, D)),
        'norm_ffn': gain((DEPTH, D)),
        'gm_w_in': nrm((N_A_LAYERS, D, 2 * GM_WIDTH), D ** -0.5),
        'gm_b_in': nrm((N_A_LAYERS, 2 * GM_WIDTH), 0.02),
        'gm_v_gain': gain((N_A_LAYERS, GM_WIDTH)),
        'gm_w_s': nrm((N_A_LAYERS, GM_GROUPS, CHUNK, CHUNK), CHUNK ** -0.5),
        'gm_b_s': gain((N_A_LAYERS, GM_GROUPS, CHUNK)),
        'gm_w_out': nrm((N_A_LAYERS, GM_WIDTH, D), GM_WIDTH ** -0.5),
        'at_w_qkv': nrm((N_B_LAYERS, D, qkv_w), D ** -0.5),
        'at_q_gain': gain((N_B_LAYERS, HEAD_DIM)),
        'at_k_gain': gain((N_B_LAYERS, HEAD_DIM)),
        'at_w_o': nrm((N_B_LAYERS, N_Q_HEADS * HEAD_DIM, D), (N_Q_HEADS * HEAD_DIM) ** -0.5),
        'moe_router_w': nrm((DEPTH, D, N_EXPERTS), D ** -0.5),
        'moe_router_b': nrm((DEPTH, N_EXPERTS), 0.01),
        'moe_w_gu': nrm((DEPTH, N_EXPERTS, D, 2 * D_EXPERT), D ** -0.5),
        'moe_b_gu': nrm((DEPTH, N_EXPERTS, 2 * D_EXPERT), 0.02),
        'moe_w_down': nrm((DEPTH, N_EXPERTS, D_EXPERT, D), D_EXPERT ** -0.5),
        'moe_b_down': nrm((DEPTH, N_EXPERTS, D), 0.02),
    }


def reference(x, c, ctx, c_ctx, ada_w, ada_b, norm_mix, norm_ffn,
              gm_w_in, gm_b_in, gm_v_gain, gm_w_s, gm_b_s, gm_w_out,
              at_w_qkv, at_q_gain, at_k_gain, at_w_o,
              moe_router_w, moe_router_b, moe_w_gu, moe_b_gu, moe_w_down, moe_b_down):
    B_, n_lat, D = x.shape
    n_ctx = ctx.shape[1]
    rows = n_lat // GRID_W
    cos, sin = axial_rope_tables(rows)
    silu_c = jax.nn.silu(c)
    silu_cc = jax.nn.silu(c_ctx)
    x_lat, x_ctx = x, ctx
    for i in range(DEPTH):
        last = i == DEPTH - 1
        mod_l = jnp.split(silu_c @ ada_w[i] + ada_b[i], N_MOD, axis=-1)
        mod_l = [m[:, None, :] for m in mod_l]
        mod_c = jnp.split(silu_cc @ ada_w[i] + ada_b[i], N_MOD, axis=-1)
        sh1, sc1, g1, sh2, sc2, g2 = mod_l
        csh1, csc1, cg1, csh2, csc2, cg2 = mod_c
        h_l = modulate(rmsnorm(x_lat, norm_mix[i]), sh1, sc1)
        h_c = modulate(rmsnorm(x_ctx, norm_mix[i]), csh1, csc1)
        if i % N_MIXERS == 0:
            a = i // N_MIXERS
            y_l = chunk_gmlp(h_l, gm_w_in[a], gm_b_in[a], gm_v_gain[a], gm_w_s[a], gm_b_s[a], gm_w_out[a])
            y_c = None if last else chunk_gmlp(h_c, gm_w_in[a], gm_b_in[a], gm_v_gain[a],
                                               gm_w_s[a], gm_b_s[a], gm_w_out[a])
        else:
            b = i // N_MIXERS
            y_l, y_c = gqa_mixer(h_l, h_c, at_w_qkv[b], at_q_gain[b], at_k_gain[b], at_w_o[b],
                                 cos, sin, not last)
        x_lat = x_lat + g1 * y_l
        if not last:
            x_ctx = x_ctx + cg1 * y_c
        f_l = modulate(rmsnorm(x_lat, norm_ffn[i]), sh2, sc2).reshape(B_ * n_lat, D)
        if last:
            tokens = f_l
        else:
            f_c = modulate(rmsnorm(x_ctx, norm_ffn[i]), csh2, csc2).reshape(B_ * n_ctx, D)
            tokens = jnp.concatenate([f_c, f_l], axis=0)
        y = moe_ffn(tokens, moe_router_w[i], moe_router_b[i], moe_w_gu[i], moe_b_gu[i],
                    moe_w_down[i], moe_b_down[i])
        if last:
            x_lat = x_lat + g2 * y.reshape(B_, n_lat, D)
        else:
            x_ctx = x_ctx + cg2 * y[:B_ * n_ctx].reshape(B_, n_ctx, D)
            x_lat = x_lat + g2 * y[B_ * n_ctx:].reshape(B_, n_lat, D)
    return x_lat
```

```python
import contextlib
import numpy as np
import concourse.bass as bass
import concourse.mybir as mybir
from concourse.bass_utils import run_bass_kernel_spmd

F32 = mybir.dt.float32
BF16 = mybir.dt.bfloat16
AF = mybir.ActivationFunctionType
ALU = mybir.AluOpType
AX = mybir.AxisListType

D = 1024
NT = 18
EPS = 1e-6
NEXP = 32
ALPHA = 1.702
LIMIT = 7.0
GR = 256
DBG = {}


class _Op:
    __slots__ = ("eng", "fn", "deps", "dma", "semkey", "signal", "ordinal", "idx")


class Prog:
    ENGS = ("pe", "act", "dve", "pool", "sp")

    def __init__(self):
        self.ops = []
        self.last_w = {}
        self.readers = {}

    def op(self, eng, fn, reads=(), writes=(), dma=None):
        o = _Op()
        o.eng, o.fn, o.dma, o.idx = eng, fn, dma is not None, len(self.ops)
        o.semkey = ("dma", dma) if dma is not None else ("eng", eng)
        o.signal = False
        o.ordinal = 0
        deps = {}
        last_w, readers = self.last_w, self.readers
        for r in reads:
            w = last_w.get(r)
            if w is not None:
                deps[w] = True
        for wr in writes:
            w = last_w.get(wr)
            if w is not None and w not in deps:
                deps[w] = False
            rd = readers.get(wr)
            if rd:
                for i in rd.values():
                    if i not in deps:
                        deps[i] = False
        o.deps = deps
        rk = (eng, o.idx) if o.dma else eng
        for r in reads:
            d = readers.get(r)
            if d is None:
                readers[r] = {rk: o.idx}
            else:
                d[rk] = o.idx
        for wr in writes:
            last_w[wr] = o.idx
            readers[wr] = None
        self.ops.append(o)
        return o

    def finalize(self):
        ops = self.ops
        for o in ops:
            keep = {}
            for d, raw in o.deps.items():
                a = ops[d]
                if (not a.dma) and (not o.dma) and a.eng == o.eng:
                    if a.eng == "pe":
                        continue
                keep[d] = raw
            o.deps = keep
            for d in keep:
                ops[d].signal = True
        counters = {}
        for o in ops:
            if o.signal:
                inc = 16 if o.dma else 1
                counters[o.semkey] = counters.get(o.semkey, 0) + inc
                o.ordinal = counters[o.semkey]
        return counters

    def emit(self, block, sems):
        ops = self.ops
        per_eng = {e: [o for o in ops if o.eng == e] for e in self.ENGS}

        def run(engobj, lst):
            waited = {}
            for o in lst:
                need = {}
                for d in o.deps:
                    a = ops[d]
                    if need.get(a.semkey, 0) < a.ordinal:
                        need[a.semkey] = a.ordinal
                for k, v in need.items():
                    if waited.get(k, 0) < v:
                        engobj.wait_ge(sems[k], v)
                        waited[k] = v
                if o.fn is None:
                    continue
                ins = o.fn(engobj)
                if o.signal:
                    ins.then_inc(sems[o.semkey], 16 if o.dma else 1)

        @block.tensor
        def _(e):
            run(e, per_eng["pe"])

        @block.scalar
        def _(e):
            run(e, per_eng["act"])

        @block.vector
        def _(e):
            run(e, per_eng["dve"])

        @block.gpsimd
        def _(e):
            run(e, per_eng["pool"])

        @block.sync
        def _(e):
            run(e, per_eng["sp"])


class Buf:
    def __init__(self, region, base_ap_f32, off, shape, dt, parts=128):
        self.region, self.off, self.shape, self.dt = region, off, list(shape), dt
        self.esz = 2 if dt == BF16 else 4
        n = 1
        for s in shape:
            n *= s
        self.nbytes = n * self.esz
        assert off % 4 == 0 and self.nbytes % 4 == 0
        flat = base_ap_f32[0:parts, off // 4:(off + self.nbytes) // 4]
        if dt != F32:
            flat = flat.bitcast(dt)
        if len(shape) == 1:
            self.ap = flat
        elif len(shape) == 2:
            self.ap = flat.rearrange("p (a b) -> p a b", a=shape[0])
        elif len(shape) == 3:
            self.ap = flat.rearrange("p (a b c) -> p a b c", a=shape[0], b=shape[1])
        else:
            raise ValueError

    def res(self, lo=0, n=None):
        if n is None:
            n = self.nbytes // self.esz - lo
        b0 = self.off + lo * self.esz
        b1 = b0 + n * self.esz
        gr = 2048 if self.region == "PS" else GR
        return [(self.region, g) for g in range(b0 // gr, (b1 - 1) // gr + 1)]

    def sub(self, i, cnt=1):
        inner = self.nbytes // self.esz // self.shape[0]
        return self.res(i * inner, cnt * inner)


def build_program(stop_after="all"):
    nc = bass.Bass("TRN2", target_bir_lowering=False)
    P = Prog()
    dbg = stop_after != "all"

    def din(name, shape, dt=F32):
        return nc.dram_tensor(name, list(shape), dt, kind="ExternalInput").ap()

    x_d = din("x", [2048, D])
    ctx_d = din("ctx", [256, D])
    cvec_d = din("cvec", [128, 16])
    ada_w_d = din("ada_w", [2, D, 6 * D])
    ada_b_d = din("ada_b", [96, 128])
    norm_d = din("norms", [32, 128])
    gm_w_in_d = din("gm_w_in", [D, 4096])
    gm_b_in_d = din("gm_b_in", [32, 128])
    gm_v_gain_d = din("gm_v_gain", [16, 128])
    gm_w_s_d = din("gm_w_s", [8, 128, 128])
    gm_b_s_d = din("gm_b_s", [1, 1024])
    gm_w_out_d = din("gm_w_out", [2048, D])
    at_w_qkv_d = din("at_w_qkv", [D, 1536])
    at_qk_gain_d = din("at_qk_gain", [1, 256])
    at_w_o_d = din("at_w_o", [D, D])
    r_w_d = din("router_w", [2, D, NEXP])
    r_b_d = din("router_b", [2, 1, NEXP])
    b_dn_d = din("b_down", [2, NEXP, D])
    ident_d = din("ident", [128, 128])
    WG_d = din("WG", [2 * NEXP * 128, 8 * 1024])
    WU_d = din("WU", [2 * NEXP * 128, 8 * 1024])
    WD_d = din("WD", [2 * NEXP * 128, 8 * 1024])
    BGL_d = din("BGL", [2 * NEXP * 128, 16])
    cU0_d = din("cU0", [128, 128])
    cU1_d = din("cU1", [128, 128])
    cIota_d = din("cIota", [128, 128])
    cPidx_d = din("cPidx", [128, 2])
    NBMAX = 50
    xs_d = nc.dram_tensor("xs_scr", [NBMAX * 512, D], BF16, kind="Internal").ap()
    ys_d = nc.dram_tensor("ys_scr", [NBMAX * 512, D], BF16, kind="Internal").ap()
    cos_d = din("rope_cos", [2048, 64])
    sin_d = din("rope_sin", [2048, 64])
    out_d = nc.dram_tensor("out", [NT * 128 if dbg else 2048, D], F32, kind="ExternalOutput").ap()

    es = contextlib.ExitStack()
    XB, SB_, KB = 73728, 133376, 5376
    Xt = es.enter_context(nc.sbuf_tensor("Xreg", [128, XB // 4], F32))
    St = es.enter_context(nc.sbuf_tensor("Sreg", [128, SB_ // 4], F32))
    Kt = es.enter_context(nc.sbuf_tensor("Kreg", [128, KB // 4], F32))
    PSt = es.enter_context(nc.psum_tensor("PSreg", [128, 4096], F32))

    def xb(off, shape, dt=F32):
        return Buf("X", Xt, off, shape, dt)

    def sbuf(off, shape, dt=F32, parts=128):
        assert off + Buf("S", St, off, shape, dt, parts).nbytes <= SB_, (off, shape)
        return Buf("S", St, off, shape, dt, parts)

    koff = [0]

    def kbuf(shape, dt=F32):
        b = Buf("K", Kt, koff[0], shape, dt)
        koff[0] += (b.nbytes + GR - 1) // GR * GR
        assert koff[0] <= KB
        return b

    def psb(bank, shape, dt=F32, off=0):
        return Buf("PS", PSt, bank * 2048 + off, shape, dt)

    X = xb(0, [NT, D])
    identb = kbuf([128], BF16)
    identf = kbuf([128])
    onesb = kbuf([128], BF16)
    MOD = kbuf([2, 48, 2])
    NRM = kbuf([4, 8])
    SIL = kbuf([8, 2])
    AB = kbuf([2, 2, 8])
    ssq = kbuf([NT])
    rstd = kbuf([NT])
    small = kbuf([64])
    BIN = kbuf([32])
    VG = kbuf([16])
    ZB = kbuf([256], BF16)
    SILb = kbuf([8, 2], BF16)

    uniq = [0]

    def dma(eng, out_ap, in_ap, reads=(), writes=(), key="misc"):
        if key in ("const", "constp", "misc"):
            uniq[0] += 1
            key = ("u", uniq[0])
        return P.op(eng, lambda e: e.dma_start(out=out_ap, in_=in_ap), reads=reads, writes=writes, dma=key)

    def mm_group(out_ap, pairs, reads, writes, fp32=False):
        n = len(pairs)

        def fn(e):
            ins = None
            for i, (l, r) in enumerate(pairs):
                ins = e.matmul(out_ap, lhsT=l, rhs=r, start=(i == 0), stop=(i == n - 1))
            return ins
        return P.op("pe", fn, reads=reads, writes=writes)

    def act(out_ap, in_ap, func, reads, writes, **kw):
        return P.op("act", lambda e: e.activation(out=out_ap, in_=in_ap, func=func, **kw), reads=reads, writes=writes)

    def dve(fn, reads, writes):
        return P.op("dve", fn, reads=reads, writes=writes)

    stage = sbuf(SB_ - 1024, [128])
    stage2 = sbuf(SB_ - 512, [128])

    def load_T(src_ap, n, dst_ap, dst_res, ps, stg):
        dma("sp", stg.ap[0:n, :], src_ap, writes=stg.res(), key=("stage", stg.off))
        mm_group(ps.ap[:, 0:n], [(stg.ap[0:n, :], identf.ap[0:n, 0:n])], reads=stg.res() + identf.res(), writes=ps.res(0, n))
        dve(lambda e: e.tensor_copy(out=dst_ap, in_=ps.ap[:, 0:n]), reads=ps.res(0, n), writes=dst_res)

    dma("sp", identf.ap, ident_d, writes=identf.res(), key="const")
    dma("pool", identb.ap, ident_d, writes=identb.res(), key="constp")
    P.op("pool", lambda e: e.memset(onesb.ap, 1.0), writes=onesb.res())
    cv = sbuf(0, [16])
    dma("sp", cv.ap, cvec_d, writes=cv.res(), key="const")
    act(SIL.ap.rearrange("p a b -> p (a b)"), cv.ap, AF.Silu, reads=cv.res(), writes=SIL.res())
    for t in range(NT):
        src = x_d[t * 128:(t + 1) * 128, :] if t < 16 else ctx_d[(t - 16) * 128:(t - 15) * 128, :]
        dma("sp", X.ap[:, t, :], src, writes=X.sub(t), key=("xload", t))

    ps0 = psb(0, [512])
    load_T(norm_d, 32, NRM.ap.rearrange("p a b -> p (a b)"), NRM.res(), ps0, stage)
    dve(lambda e: e.tensor_copy(out=SILb.ap, in_=SIL.ap), reads=SIL.res(), writes=SILb.res())
    pm = psb(2, [2, 48, 2])

    def adaln_dma(l, blk, w, ncol):
        dma("pool", w.ap, ada_w_d[l, :, blk * ncol:(blk + 1) * ncol].rearrange("(k p) n -> p k n", p=128),
            writes=w.res(), key=("adaw", w.off))

    def adaln_mm(l, blk, w, ncol):
        for m in range(ncol // 128):
            j = blk * (ncol // 128) + m
            mm_group(pm.ap[:, l, j, :], [(w.ap[:, k, m * 128:(m + 1) * 128], SILb.ap[:, k, :]) for k in range(8)],
                     reads=w.res() + SILb.res(), writes=pm.res())

    def adaln_fin(l):
        for c in range(2):
            dve(lambda e, c=c: e.tensor_tensor(out=MOD.ap[:, l, :, c], in0=pm.ap[:, l, :, c],
                                               in1=adab.ap[:, l * 48:(l + 1) * 48], op=ALU.add),
                reads=pm.res() + adab.res(), writes=MOD.res())

    def adaln(l, wbufs, ncol):
        for blk in range(6144 // ncol):
            w = wbufs[blk % len(wbufs)]
            adaln_dma(l, blk, w, ncol)
            adaln_mm(l, blk, w, ncol)
        adaln_fin(l)

    adab = kbuf([96])
    load_T(ada_b_d, 96, adab.ap, adab.res(), psb(1, [512]), stage2)
    adaln(0, [sbuf(4096 + i * 8192, [8, 512], BF16) for i in range(2)], 512)

    def build_AB(l, which, nrm_idx):
        n_sh, n_sc = (0, 1) if which == 0 else (3, 4)
        for c in range(2):
            dve(lambda e, c=c: e.scalar_tensor_tensor(out=AB.ap[:, c, 0, :], in0=MOD.ap[:, l, n_sc * 8:(n_sc + 1) * 8, c],
                                                      scalar=1.0, in1=NRM.ap[:, nrm_idx, :], op0=ALU.add, op1=ALU.mult),
                reads=MOD.res() + NRM.res(), writes=AB.res())
            dve(lambda e, c=c: e.tensor_copy(out=AB.ap[:, c, 1, :], in_=MOD.ap[:, l, n_sh * 8:(n_sh + 1) * 8, c]),
                reads=MOD.res(), writes=AB.res())

    def build_gbc(l, n_g, c, dst, ps):
        rep = sbuf(SB_ - 2048, [2, 128])
        for k in range(8):
            r = rep.ap[:, k % 2, :]
            dve(lambda e, k=k, r=r: e.tensor_scalar(out=r, in0=identf.ap, scalar1=0.0, scalar2=MOD.ap[:, l, n_g * 8 + k, c:c + 1],
                                                    op0=ALU.mult, op1=ALU.add),
                reads=MOD.res() + identf.res(), writes=rep.sub(k % 2))
            mm_group(ps.ap[:, (k % 4) * 128:(k % 4 + 1) * 128], [(r, identf.ap)], reads=rep.sub(k % 2) + identf.res(),
                     writes=ps.res((k % 4) * 128, 128))
            if k % 4 == 3:
                h = k // 4
                dve(lambda e, h=h: e.tensor_copy(out=dst.ap[:, h * 512:(h + 1) * 512], in_=ps.ap), reads=ps.res(), writes=dst.res(h * 512, 512))

    def phase_rstd(tiles, junk):
        for t in tiles:
            act(junk.ap, X.ap[:, t, :], AF.Square, reads=X.sub(t), writes=junk.res() + ssq.res(),
                scale=1.0 / 32.0, accum_out=ssq.ap[:, t:t + 1])
        lo, hi = min(tiles), max(tiles) + 1
        act(rstd.ap[:, lo:hi], ssq.ap[:, lo:hi], AF.Sqrt, reads=ssq.res(), writes=rstd.res(), bias=EPS, scale=1.0)
        dve(lambda e: e.reciprocal(out=rstd.ap[:, lo:hi], in_=rstd.ap[:, lo:hi]), reads=rstd.res(), writes=rstd.res())

    def norm_T(t, xn, pst, dst_ap, dst_res):
        c = 0 if t < 16 else 1
        act(xn.ap, X.ap[:, t, :], AF.Copy, reads=X.sub(t) + rstd.res(), writes=xn.res(), scale=rstd.ap[:, t:t + 1])

        def tr(e):
            ins = None
            for k in range(8):
                ins = e.transpose(out=pst.ap[:, k, :], in_=xn.ap[:, k * 128:(k + 1) * 128], identity=identb.ap)
            return ins
        P.op("pe", tr, reads=xn.res() + identb.res(), writes=pst.res())

        def ev(e):
            ins = None
            for k in range(8):
                ins = e.tensor_scalar(out=dst_ap[:, k, :], in0=pst.ap[:, k, :], scalar1=AB.ap[:, c, 0, k:k + 1],
                                      scalar2=AB.ap[:, c, 1, k:k + 1], op0=ALU.mult, op1=ALU.add)
            return ins
        dve(ev, reads=pst.res() + AB.res(), writes=dst_res)

    def resid_add(t, nh, ps, gbc, tmp):
        dve(lambda e: e.tensor_tensor(out=tmp.ap, in0=ps.ap, in1=gbc.ap[:, nh * 512:(nh + 1) * 512], op=ALU.mult),
            reads=ps.res() + gbc.res(nh * 512, 512), writes=tmp.res())
        xs = X.ap[:, t, nh * 512:(nh + 1) * 512]
        xr = X.res(t * D + nh * 512, 512)
        dve(lambda e: e.tensor_tensor(out=xs, in0=xs, in1=tmp.ap, op=ALU.add), reads=xr + tmp.res(), writes=xr)

    slab_ctr = [0]

    def load_slab(dst, src_ap):
        slab_ctr[0] += 1
        dma("pool", dst.ap, src_ap, writes=dst.res(), key=("slab", dst.off))

    def gmlp_phase():
        l = 0
        o = 0
        slabs = []
        for i in range(6):
            slabs.append(sbuf(o, [8, 1024], BF16)); o += 16384
        bsbc = sbuf(o, [8, 128]); o += 4096
        xn = sbuf(o, [1024], BF16); o += 2048
        hT = [sbuf(o, [8, 128], BF16)] * 2; o += 2048
        uvT = sbuf(o, [32, 128], BF16); o += 8192
        vn = sbuf(o, [2048], BF16); o += 4096
        usT = sbuf(o, [16, 128], BF16); o += 4096
        tmpS = sbuf(o, [4, 128])
        tmpY = sbuf(o, [512])
        junk = sbuf(o, [1024], BF16); o += 2048
        gbc1 = sbuf(o, [1024]); o += 4096
        gbc = [gbc1, gbc1]
        wsT = sbuf(o, [8, 128], BF16); o += 2048
        wsl = stage2
        vss = small
        for i in range(4):
            load_slab(slabs[i], gm_w_in_d[:, i * 1024:(i + 1) * 1024].rearrange("(k p) n -> p k n", p=128))
        for i in range(2):
            load_slab(slabs[4 + i], gm_w_out_d[i * 1024:(i + 1) * 1024, :].rearrange("(k p) n -> p k n", p=128))
        dma("sp", bsbc.ap.rearrange("p a b -> p (a b)"), gm_b_s_d.partition_broadcast(128), writes=bsbc.res(), key="const")
        P.op("pool", lambda e: e.memset(ZB.ap, 0.0), writes=ZB.res())
        zsrc = ZB.ap.unsqueeze(1).to_broadcast([128, 64, 256])
        for zi in range(NBMAX * 512 // 2048 + (1 if (NBMAX * 512) % 2048 else 0)):
            r0 = zi * 2048
            r1 = min(r0 + 2048, NBMAX * 512)
            na = (r1 - r0) // 128
            dma("pool", xs_d[r0:r1, :].rearrange("(p a) (b c) -> p (a b) c", p=128, c=256), ZB.ap.unsqueeze(1).to_broadcast([128, na * 4, 256]),
                reads=ZB.res(), writes=[("xsz", zi)], key="zero")
        load_T(gm_b_in_d, 32, BIN.ap, BIN.res(), psb(0, [512]), stage)
        load_T(gm_v_gain_d, 16, VG.ap, VG.res(), psb(1, [512]), stage2)
        for g in range(8):
            dma("sp", wsl.ap, gm_w_s_d[g], writes=wsl.res(), key=("stage", wsl.off))
            pw = psb(g % 2, [128])
            mm_group(pw.ap, [(wsl.ap, identf.ap)], reads=wsl.res() + identf.res(), writes=pw.res())
            dve(lambda e, g=g, pw=pw: e.tensor_copy(out=wsT.ap[:, g, :], in_=pw.ap), reads=pw.res(), writes=wsT.sub(g))
        build_AB(l, 0, 0)
        build_gbc(l, 2, 0, gbc[0], psb(2, [512]))
        phase_rstd(list(range(NT)), junk)
        for t in range(NT):
            c = 0 if t < 16 else 1
            if t == 16:
                build_gbc(l, 2, 1, gbc[1], psb(5, [512]))
            h = hT[t % 2]
            if t == 0:
                norm_T(t, xn, psb(4, [8, 128], BF16), h.ap, h.res())
            for jb in range(8):
                pus = []
                for m in range(4):
                    j = jb * 4 + m
                    pu = psb(jb % 2, [128], F32, off=m * 512)
                    pus.append(pu)
                    sl = slabs[j // 8]
                    c0 = (j % 8) * 128
                    mm_group(pu.ap, [(sl.ap[:, k, c0:c0 + 128], h.ap[:, k, :]) for k in range(8)],
                             reads=sl.res() + h.res(), writes=pu.res())
                for m in range(4):
                    j = jb * 4 + m
                    act(uvT.ap[:, j, :], pus[m].ap, AF.Gelu, reads=pus[m].res() + BIN.res(), writes=uvT.sub(j), bias=BIN.ap[:, j:j + 1], scale=1.0)
            pv = [psb(2 + r, [8, 128], BF16) for r in range(2)]
            for r in range(2):
                def trv(e, r=r):
                    ins = None
                    for i in range(8):
                        ins = e.transpose(out=pv[r].ap[:, i, :], in_=uvT.ap[:, 16 + r * 8 + i, :], identity=identb.ap)
                    return ins
                P.op("pe", trv, reads=uvT.sub(16 + r * 8, 8) + identb.res(), writes=pv[r].res())
                act(junk.ap, pv[r].ap.rearrange("p a b -> p (a b)"), AF.Square, reads=pv[r].res(), writes=junk.res() + vss.res(),
                    accum_out=vss.ap[:, r:r + 1])
            dve(lambda e: e.tensor_tensor(out=vss.ap[:, 2:3], in0=vss.ap[:, 0:1], in1=vss.ap[:, 1:2], op=ALU.add), reads=vss.res(), writes=vss.res())
            act(vss.ap[:, 3:4], vss.ap[:, 2:3], AF.Sqrt, reads=vss.res(), writes=vss.res(), bias=EPS, scale=1.0 / 2048.0)
            dve(lambda e: e.reciprocal(out=vss.ap[:, 4:5], in_=vss.ap[:, 3:4]), reads=vss.res(), writes=vss.res())
            for r in range(2):
                dve(lambda e, r=r: e.tensor_scalar(out=vn.ap[:, r * 1024:(r + 1) * 1024], in0=pv[r].ap.rearrange("p a b -> p (a b)"),
                                                   scalar1=vss.ap[:, 4:5], scalar2=None, op0=ALU.mult),
                    reads=pv[r].res() + vss.res(), writes=vn.res(r * 1024, 1024))
            for q4 in range(4):
                pS = psb(4 + q4 % 2, [4, 128])
                for i in range(4):
                    jj = q4 * 4 + i
                    mm_group(pS.ap[:, i, :], [(vn.ap[:, jj * 128:(jj + 1) * 128], wsT.ap[:, jj // 2, :])],
                             reads=vn.res(jj * 128, 128) + wsT.sub(jj // 2), writes=pS.sub(i))

                def sg(e, q4=q4, pS=pS):
                    ins = None
                    for i in range(4):
                        jj = q4 * 4 + i
                        ins = e.scalar_tensor_tensor(out=tmpS.ap[:, i, :], in0=pS.ap[:, i, :], scalar=VG.ap[:, jj:jj + 1],
                                                     in1=bsbc.ap[:, jj // 2, :], op0=ALU.mult, op1=ALU.add)
                    return ins
                dve(sg, reads=pS.res() + VG.res() + bsbc.res(), writes=tmpS.res())
                dve(lambda e, q4=q4: e.tensor_tensor(out=usT.ap[:, q4 * 4:(q4 + 1) * 4, :], in0=tmpS.ap, in1=uvT.ap[:, q4 * 4:(q4 + 1) * 4, :], op=ALU.mult),
                    reads=tmpS.res() + uvT.sub(q4 * 4, 4), writes=usT.sub(q4 * 4, 4))
            if t + 1 < NT:
                if t + 1 == 16:
                    pass
                norm_T(t + 1, xn, psb(4, [8, 128], BF16), hT[(t + 1) % 2].ap, hT[(t + 1) % 2].res())
            for nh in range(2):
                py = psb(6 + nh, [512])
                mm_group(py.ap, [(usT.ap[:, jj, :], slabs[4 + jj // 8].ap[:, jj % 8, nh * 512:(nh + 1) * 512]) for jj in range(16)],
                         reads=usT.res() + slabs[4].res() + slabs[5].res(), writes=py.res())
                resid_add(t, nh, py, gbc[c], tmpY)

    I32 = mybir.dt.int32
    IOA = bass.IndirectOffsetOnAxis

    def moe_sparse_phase(l, tiles):
        T = len(tiles)
        NB = T + 31
        assert NB <= NBMAX
        o = 0
        ring = []
        for i in range(5):
            ring.append(sbuf(o, [8, 1024], BF16)); o += 16384
        xtok = sbuf(o, [4, 1024], BF16); o += 8192
        fTb = sbuf(o, [8, 512], BF16); o += 8192
        actT = sbuf(o, [8, 512], BF16); o += 8192
        yblk = sbuf(o, [4, 1024], BF16); o += 8192
        tg = sbuf(o, [512]); o += 2048
        tsg = sbuf(o, [512]); o += 2048
        tu = sbuf(o, [512]); o += 2048
        bgb = [sbuf(o + i * 256, [16]) for i in range(2)]; o += 512
        G = sbuf(o, [NT, NEXP]); o += 2304
        LG = sbuf(o, [NT, NEXP]); o += 2304
        M8 = sbuf(o, [NT, 8]); o += 768
        GATE4 = sbuf(o, [NT, 4]); o += 512
        SLOTF = sbuf(o, [NT, 4]); o += 512
        SLOTI = sbuf(o, [NT, 4], I32); o += 512
        WIDX = sbuf(o, [64], I32); o += 256
        assert o <= SB_ - 2048, o
        r = 5 * 16384
        U0 = sbuf(r, [128]); r += 512
        U1 = sbuf(r, [128]); r += 512
        IOTA = sbuf(r, [128]); r += 512
        ONESF = sbuf(r, [128]); r += 512
        PIDX = sbuf(r, [2]); r += 256
        abc = [sbuf(r + i * 4096, [1024]) for i in range(2)]; r += 8192
        bbc = [sbuf(r + i * 4096, [1024]) for i in range(2)]; r += 8192
        ftok = [sbuf(r + i * 2048, [1024], BF16) for i in range(4)]; r += 8192
        tmp32 = sbuf(r, [1024]); r += 4096
        MALL = sbuf(r, [NT, NEXP]); r += 2304
        CP = sbuf(r, [NEXP]); r += 256
        IOTD = sbuf(r, [NEXP]); r += 256
        SL = sbuf(r, [NEXP]); r += 256
        j32 = sbuf(r, [NEXP]); r += 256
        cnt = sbuf(r, [64]); r += 256
        cmp8 = sbuf(r, [8]); r += 256
        diag = sbuf(r, [NEXP]); r += 256
        BSB = sbuf(r, [NEXP]); r += 256
        cmpb = sbuf(r, [64]); r += 256
        bef = sbuf(r, [64]); r += 256
        rbbc = sbuf(r, [NEXP]); r += 256
        assert r <= bgb[0].off + 512, r
        q = 5 * 16384
        q -= 2048; fTt = sbuf(q, [8, 128], BF16)
        q -= 2048; xn = sbuf(q, [1024], BF16)
        q -= 2048; junk = sbuf(q, [1024], BF16)
        q -= 512; RW = sbuf(q, [8, NEXP], BF16)
        q -= 256; ex = sbuf(q, [NEXP])
        q -= 256; sm = sbuf(q, [64])
        wada = []
        for i in range(3):
            q -= 8192; wada.append(sbuf(q, [8, 512], BF16))
        assert q >= 3 * 16384, q
        q = 0
        gbc = [sbuf(q + i * 4096, [1024]) for i in range(2)]; q += 8192
        bdn = sbuf(q, [1024], BF16, parts=32); q += 2048
        Gb = sbuf(q, [NEXP], BF16); q += 256
        GT = sbuf(q, [128], BF16, parts=32); q += 256
        ytok = [sbuf(q + i * 8192, [4, 1024], BF16) for i in range(2)]; q += 16384
        tmpY = sbuf(q, [512]); q += 2048
        dg = [sbuf(q + i * 2048, [8, 128], BF16) for i in range(2)]; q += 4096
        g4 = sbuf(q, [16]); q += 256
        g4b = sbuf(q, [4], BF16); q += 256
        assert q <= 5 * 16384, q
        lbase = float(l * NEXP * 128)

        dma("sp", U0.ap, cU0_d, writes=U0.res(), key="const")
        dma("sp", U1.ap, cU1_d, writes=U1.res(), key="const")
        dma("sp", IOTA.ap, cIota_d, writes=IOTA.res(), key="const")
        dma("sp", PIDX.ap, cPidx_d, writes=PIDX.res(), key="const")
        dve(lambda e: e.memset(ONESF.ap, 1.0), reads=(), writes=ONESF.res())
        dma("pool", RW.ap, r_w_d[l].rearrange("(k p) n -> p k n", p=128), writes=RW.res(), key="const")
        dma("sp", rbbc.ap, r_b_d[l].partition_broadcast(128), writes=rbbc.res(), key="const")
        dve(lambda e: e.tensor_scalar(out=IOTD.ap, in0=IOTA.ap[:, 0:NEXP], scalar1=2.0 ** -20, scalar2=None, op0=ALU.mult), reads=IOTA.res(), writes=IOTD.res())
        dve(lambda e: e.tensor_tensor(out=IOTD.ap, in0=rbbc.ap, in1=IOTD.ap, op=ALU.subtract), reads=rbbc.res() + IOTD.res(), writes=IOTD.res())
        build_AB(l, 1, 2 + l)
        ncls = 2 if l == 0 else 1
        for c in range(ncls):
            for which, dst in ((0, abc[c]), (1, bbc[c])):
                rep = sbuf(SB_ - 2048, [2, 128])
                ps = psb(c * 2 + which, [512])
                for k in range(8):
                    r = rep.ap[:, k % 2, :]
                    dve(lambda e, k=k, r=r, c=c, which=which: e.tensor_scalar(out=r, in0=identf.ap, scalar1=0.0, scalar2=AB.ap[:, c, which, k:k + 1],
                                                                             op0=ALU.mult, op1=ALU.add),
                        reads=AB.res() + identf.res(), writes=rep.sub(k % 2))
                    mm_group(ps.ap[:, (k % 4) * 128:(k % 4 + 1) * 128], [(r, identf.ap)], reads=rep.sub(k % 2) + identf.res(),
                             writes=ps.res((k % 4) * 128, 128))
                    if k % 4 == 3:
                        h = k // 4
                        dve(lambda e, h=h, dst=dst, ps=ps: e.tensor_copy(out=dst.ap[:, h * 512:(h + 1) * 512], in_=ps.ap), reads=ps.res(), writes=dst.res(h * 512, 512))
        phase_rstd(tiles, junk)

        for it1, t in enumerate(tiles):
            c = 0 if t < 16 else 1
            if l == 0:
                if it1 < 12:
                    adaln_dma(1, it1, wada[it1 % 3], 512)
                if 2 <= it1 < 14:
                    adaln_mm(1, it1 - 2, wada[(it1 - 2) % 3], 512)
                if it1 == 14:
                    adaln_fin(1)
            norm_T(t, xn, psb(7, [8, 128], BF16), fTt.ap, fTt.res())
            pl = psb(6, [NEXP])
            mm_group(pl.ap, [(fTt.ap[:, k, :], RW.ap[:, k, :]) for k in range(8)], reads=fTt.res() + RW.res(), writes=pl.res())
            lgt = LG.ap[:, t, :]
            dve(lambda e, pl=pl, lgt=lgt: e.tensor_tensor(out=lgt, in0=pl.ap, in1=IOTD.ap, op=ALU.add), reads=pl.res() + IOTD.res(), writes=LG.sub(t))
            dve(lambda e, t=t, lgt=lgt: e.max(out=M8.ap[:, t, :], in_=lgt), reads=LG.sub(t), writes=M8.sub(t))
            dve(lambda e, t=t: e.tensor_scalar(out=sm.ap[:, 0:1], in0=M8.ap[:, t, 0:1], scalar1=-1.0, scalar2=None, op0=ALU.mult), reads=M8.sub(t), writes=sm.res())
            act(ex.ap, lgt, AF.Exp, reads=LG.sub(t) + sm.res(), writes=ex.res(), bias=sm.ap[:, 0:1], scale=1.0)
            act(sm.ap[:, 4:8], M8.ap[:, t, 0:4], AF.Exp, reads=M8.sub(t) + sm.res(), writes=sm.res(), bias=sm.ap[:, 0:1], scale=1.0)
            dve(lambda e, t=t, lgt=lgt: e.tensor_scalar(out=MALL.ap[:, t, :], in0=lgt, scalar1=M8.ap[:, t, 3:4], scalar2=None, op0=ALU.is_ge),
                reads=LG.sub(t) + M8.sub(t), writes=MALL.sub(t))
            dve(lambda e, t=t: e.tensor_tensor(out=ex.ap, in0=ex.ap, in1=MALL.ap[:, t, :], op=ALU.mult), reads=ex.res() + MALL.sub(t), writes=ex.res())
            dve(lambda e: e.tensor_reduce(out=sm.ap[:, 1:2], in_=ex.ap, axis=AX.X, op=ALU.add), reads=ex.res(), writes=sm.res())
            dve(lambda e: e.reciprocal(out=sm.ap[:, 2:3], in_=sm.ap[:, 1:2]), reads=sm.res(), writes=sm.res())
            dve(lambda e, t=t: e.tensor_scalar(out=G.ap[:, t, :], in0=ex.ap, scalar1=sm.ap[:, 2:3], scalar2=None, op0=ALU.mult),
                reads=ex.res() + sm.res(), writes=G.sub(t))
            dve(lambda e, t=t: e.tensor_scalar(out=GATE4.ap[:, t, :], in0=sm.ap[:, 4:8], scalar1=sm.ap[:, 2:3], scalar2=None, op0=ALU.mult),
                reads=sm.res(), writes=GATE4.sub(t))

        pn = psb(0, [512])
        n_t = len(tiles)

        def cntfn(e):
            ins = None
            for i, t in enumerate(tiles):
                ins = e.matmul(pn.ap[0:32, 0:1], lhsT=MALL.ap[:, t, :], rhs=ONESF.ap[:, 0:1], start=(i == 0), stop=(i == n_t - 1))
            return ins
        P.op("pe", cntfn, reads=MALL.res() + ONESF.res(), writes=pn.res())
        c32 = cnt.ap[0:32, :]
        dve(lambda e: e.tensor_copy(out=c32[:, 0:1], in_=pn.ap[0:32, 0:1]), reads=pn.res(), writes=cnt.res())
        dve(lambda e: e.tensor_scalar(out=cmp8.ap[0:32, :], in0=IOTA.ap[0:32, 0:8], scalar1=512.0, scalar2=c32[:, 0:1], op0=ALU.mult, op1=ALU.is_lt),
            reads=IOTA.res() + cnt.res(), writes=cmp8.res())
        dve(lambda e: e.tensor_reduce(out=c32[:, 1:2], in_=cmp8.ap[0:32, :], axis=AX.X, op=ALU.add), reads=cmp8.res(), writes=cnt.res())
        pb = psb(1, [512])
        mm_group(pb.ap[0:32, 0:1], [(U0.ap[0:32, 0:32], c32[:, 1:2])], reads=U0.res() + cnt.res(), writes=pb.res())
        dve(lambda e: e.tensor_copy(out=c32[:, 2:3], in_=pb.ap[0:32, 0:1]), reads=pb.res(), writes=cnt.res())
        dve(lambda e: e.tensor_tensor(out=c32[:, 3:4], in0=c32[:, 2:3], in1=c32[:, 1:2], op=ALU.subtract), reads=cnt.res(), writes=cnt.res())
        dve(lambda e: e.tensor_scalar(out=c32[:, 4:5], in0=c32[:, 3:4], scalar1=512.0, scalar2=None, op0=ALU.mult), reads=cnt.res(), writes=cnt.res())
        dve(lambda e: e.tensor_scalar(out=diag.ap[0:32, :], in0=identf.ap[0:32, 0:32], scalar1=c32[:, 4:5], scalar2=None, op0=ALU.mult),
            reads=identf.res() + cnt.res(), writes=diag.res())
        pbs = psb(2, [512])
        mm_group(pbs.ap[:, 0:32], [(ONESF.ap[0:32, :], diag.ap[0:32, :])], reads=ONESF.res() + diag.res(), writes=pbs.res())
        dve(lambda e: e.tensor_copy(out=BSB.ap, in_=pbs.ap[:, 0:32]), reads=pbs.res(), writes=BSB.res())
        dve(lambda e: e.tensor_scalar(out=cmpb.ap[0:32, 0:NB], in0=IOTA.ap[0:32, 0:NB], scalar1=c32[:, 2:3], scalar2=None, op0=ALU.is_ge),
            reads=IOTA.res() + cnt.res(), writes=cmpb.res())
        pbe = psb(3, [512])
        mm_group(pbe.ap[:, 0:NB], [(ONESF.ap[0:32, :], cmpb.ap[0:32, 0:NB])], reads=ONESF.res() + cmpb.res(), writes=pbe.res())
        dve(lambda e: e.tensor_scalar(out=bef.ap[:, 0:NB], in0=pbe.ap[:, 0:NB], scalar1=31.0, scalar2=128.0, op0=ALU.min, op1=ALU.mult),
            reads=pbe.res(), writes=bef.res())
        dve(lambda e: e.tensor_scalar(out=bef.ap[:, 0:NB], in0=bef.ap[:, 0:NB], scalar1=PIDX.ap[:, 0:1], scalar2=lbase, op0=ALU.add, op1=ALU.add),
            reads=bef.res() + PIDX.res(), writes=bef.res())
        dve(lambda e: e.tensor_copy(out=WIDX.ap[:, 0:NB], in_=bef.ap[:, 0:NB]), reads=bef.res(), writes=WIDX.res())

        seq = []
        for i in range(NB):
            seq += [(i, "g"), (i, "u"), (i, "d")]
        loaded = [0]

        def ensure(upto):
            while loaded[0] <= upto and loaded[0] < len(seq):
                s_ = loaded[0]
                i, kind = seq[s_]
                dst = ring[s_ % 5]
                src = {"g": WG_d, "u": WU_d, "d": WD_d}[kind]
                P.op("pool", lambda e, dst=dst, src=src, i=i: e.indirect_dma_start(out=dst.ap.rearrange("p k n -> p (k n)"), out_offset=None, in_=src[:, :],
                                                                                 in_offset=IOA(ap=WIDX.ap[:, i:i + 1], axis=0)),
                     reads=WIDX.res(), writes=dst.res(), dma=("slab", dst.off))
                if kind == "g":
                    bb = bgb[i % 2]
                    P.op("pool", lambda e, bb=bb, i=i: e.indirect_dma_start(out=bb.ap, out_offset=None, in_=BGL_d[:, :],
                                                                           in_offset=IOA(ap=WIDX.ap[:, i:i + 1], axis=0)),
                         reads=WIDX.res(), writes=bb.res(), dma=("bgb", i % 2))
                loaded[0] += 1

        ensure(4)
        XSZ = [("xsz", zi) for zi in range(13)]
        XS_SC = [("xssc", l, it, k) for it in range(len(tiles)) for k in range(4)]
        YS_ALL = [("ys", b) for b in range(NB)]
        dve(lambda e: e.memset(CP.ap, 0.0), reads=(), writes=CP.res())
        for it, t in enumerate(tiles):
            c = 0 if t < 16 else 1
            pr = psb(4 + it % 2, [512])
            mm_group(pr.ap[:, 0:32], [(U1.ap, MALL.ap[:, t, :]), (ONESF.ap, CP.ap)], reads=U1.res() + MALL.sub(t) + ONESF.res() + CP.res(), writes=pr.res())
            dve(lambda e, pr=pr: e.tensor_tensor(out=SL.ap, in0=pr.ap[:, 0:32], in1=BSB.ap, op=ALU.add), reads=pr.res() + BSB.res(), writes=SL.res())
            dve(lambda e, t=t: e.tensor_tensor(out=CP.ap, in0=CP.ap, in1=MALL.ap[:, t, :], op=ALU.add), reads=CP.res() + MALL.sub(t), writes=CP.res())
            for k in range(4):
                dve(lambda e, t=t, k=k: e.scalar_tensor_tensor(out=j32.ap, in0=LG.ap[:, t, :], scalar=M8.ap[:, t, k:k + 1], in1=SL.ap,
                                                               op0=ALU.is_equal, op1=ALU.mult, accum_out=SLOTF.ap[:, t, k:k + 1]),
                    reads=LG.sub(t) + M8.sub(t) + SL.res(), writes=j32.res() + SLOTF.sub(t))
            dve(lambda e, t=t: e.tensor_copy(out=SLOTI.ap[:, t, :], in_=SLOTF.ap[:, t, :]), reads=SLOTF.sub(t), writes=SLOTI.sub(t))
            ft = ftok[it % 4]
            dve(lambda e, t=t, c=c: e.scalar_tensor_tensor(out=tmp32.ap, in0=X.ap[:, t, :], scalar=rstd.ap[:, t:t + 1], in1=abc[c].ap, op0=ALU.mult, op1=ALU.mult),
                reads=X.sub(t) + rstd.res() + abc[c].res(), writes=tmp32.res())
            dve(lambda e, ft=ft, c=c: e.tensor_tensor(out=ft.ap, in0=tmp32.ap, in1=bbc[c].ap, op=ALU.add), reads=tmp32.res() + bbc[c].res(), writes=ft.res())
            for k in range(4):
                P.op("pool", lambda e, ft=ft, t=t, k=k: e.indirect_dma_start(out=xs_d[0:NB * 512, :], out_offset=IOA(ap=SLOTI.ap[:, t, k:k + 1], axis=0),
                                                                            in_=ft.ap, in_offset=None),
                     reads=ft.res() + SLOTI.sub(t) + XSZ, writes=[("xssc", l, it, k)], dma=("scat", it % 4, k))

        def load_xtok(i):
            dma("sp", xtok.ap, xs_d[i * 512:(i + 1) * 512, :].rearrange("(s p) d -> p s d", p=128), reads=XS_SC, writes=xtok.res(), key="xtok")

        def transposes(i):
            for s4 in range(4):
                pt = psb(6 + s4 % 2, [8, 128], BF16)

                def trb(e, s4=s4, pt=pt):
                    ins = None
                    for k in range(8):
                        ins = e.transpose(out=pt.ap[:, k, :], in_=xtok.ap[:, s4, k * 128:(k + 1) * 128], identity=identb.ap)
                    return ins
                P.op("pe", trb, reads=xtok.sub(s4) + identb.res(), writes=pt.res())
                act(fTb.ap[:, :, s4 * 128:(s4 + 1) * 128], pt.ap, AF.Copy, reads=pt.res(), writes=fTb.res())
            if i + 1 < NB:
                load_xtok(i + 1)

        ensure(4)
        load_xtok(0)
        transposes(0)
        for i in range(NB):
            s0 = 3 * i
            ensure(s0 + 4)
            Wg, Wu, Wd = ring[s0 % 5], ring[(s0 + 1) % 5], ring[(s0 + 2) % 5]
            bb = bgb[i % 2]
            for j in range(8):
                pg = psb(j % 2, [512])
                pu = psb(2 + j % 2, [512])
                mm_group(pg.ap, [(Wg.ap[:, k, j * 128:(j + 1) * 128], fTb.ap[:, k, :]) for k in range(8)], reads=Wg.res() + fTb.res(), writes=pg.res())
                mm_group(pu.ap, [(Wu.ap[:, k, j * 128:(j + 1) * 128], fTb.ap[:, k, :]) for k in range(8)], reads=Wu.res() + fTb.res(), writes=pu.res())
                bg = bb.ap[:, j:j + 1]
                bu = bb.ap[:, 8 + j:9 + j]
                dve(lambda en, pg=pg, bg=bg: en.tensor_scalar(out=tg.ap, in0=pg.ap, scalar1=bg, scalar2=LIMIT, op0=ALU.add, op1=ALU.min),
                    reads=pg.res() + bb.res(), writes=tg.res())
                act(tsg.ap, tg.ap, AF.Silu, reads=tg.res(), writes=tsg.res(), scale=ALPHA)
                dve(lambda en, pu=pu, bu=bu: en.tensor_scalar(out=tu.ap, in0=pu.ap, scalar1=bu, scalar2=LIMIT, op0=ALU.add, op1=ALU.min),
                    reads=pu.res() + bb.res(), writes=tu.res())
                dve(lambda en: en.tensor_scalar(out=tu.ap, in0=tu.ap, scalar1=-LIMIT, scalar2=1.0, op0=ALU.max, op1=ALU.add), reads=tu.res(), writes=tu.res())
                dve(lambda en, j=j: en.scalar_tensor_tensor(out=actT.ap[:, j, :], in0=tsg.ap, scalar=1.0 / ALPHA, in1=tu.ap, op0=ALU.mult, op1=ALU.mult),
                    reads=tsg.res() + tu.res(), writes=actT.sub(j))
            ensure(s0 + 6)
            if i + 1 < NB:
                transposes(i + 1)
            for s4 in range(4):
                for nh in range(2):
                    pd = psb(4 + nh, [512])
                    mm_group(pd.ap, [(actT.ap[:, k, s4 * 128:(s4 + 1) * 128], Wd.ap[:, k, nh * 512:(nh + 1) * 512]) for k in range(8)],
                             reads=actT.res() + Wd.res(), writes=pd.res())
                    act(yblk.ap[:, s4, nh * 512:(nh + 1) * 512], pd.ap, AF.Copy, reads=pd.res(), writes=yblk.res(s4 * 1024 + nh * 512, 512))
            ensure(s0 + 7)
            dma("sp", ys_d[i * 512:(i + 1) * 512, :].rearrange("(s p) d -> p s d", p=128), yblk.ap, reads=yblk.res(), writes=[("ys", i)], key="ysst")

        build_gbc(l, 5, 0, gbc[0], psb(0, [512]))
        if l == 0:
            build_gbc(l, 5, 1, gbc[1], psb(1, [512]))
        dma("pool", bdn.ap, b_dn_d[l], writes=bdn.res(), key="const")
        for it, t in enumerate(tiles):
            c = 0 if t < 16 else 1
            yt = ytok[it % 2]
            for k in range(4):
                P.op("pool", lambda e, yt=yt, t=t, k=k: e.indirect_dma_start(out=yt.ap[:, k, :], out_offset=None, in_=ys_d[0:NB * 512, :],
                                                                            in_offset=IOA(ap=SLOTI.ap[:, t, k:k + 1], axis=0)),
                     reads=YS_ALL + SLOTI.sub(t), writes=yt.sub(k), dma=("gath", it % 2, k))
            dve(lambda e, t=t: e.tensor_copy(out=g4b.ap, in_=GATE4.ap[:, t, :]), reads=GATE4.sub(t), writes=g4b.res())
            dve(lambda e: e.tensor_copy(out=g4.ap[:, 0:4], in_=g4b.ap), reads=g4b.res(), writes=g4.res())
            dve(lambda e, t=t: e.tensor_tensor(out=g4.ap[:, 4:8], in0=GATE4.ap[:, t, :], in1=g4.ap[:, 0:4], op=ALU.subtract),
                reads=GATE4.sub(t) + g4.res(), writes=g4.res())
            dgt = dg[it % 2]

            def mkdiag(e, t=t, dgt=dgt):
                ins = None
                for k in range(4):
                    e.tensor_scalar(out=dgt.ap[:, k, :], in0=identb.ap, scalar1=GATE4.ap[:, t, k:k + 1], scalar2=None, op0=ALU.mult)
                    ins = e.tensor_scalar(out=dgt.ap[:, 4 + k, :], in0=identb.ap, scalar1=g4.ap[:, 4 + k:5 + k], scalar2=None, op0=ALU.mult)
                return ins
            dve(mkdiag, reads=identb.res() + GATE4.sub(t) + g4.res(), writes=dgt.res())
            dve(lambda e, t=t: e.tensor_copy(out=Gb.ap, in_=G.ap[:, t, :]), reads=G.sub(t), writes=Gb.res())
            pgt = psb(6, [128], BF16)
            P.op("pe", lambda e, pgt=pgt: e.transpose(out=pgt.ap[0:32, :], in_=Gb.ap, identity=identb.ap), reads=Gb.res() + identb.res(), writes=pgt.res())
            dve(lambda e, pgt=pgt: e.tensor_copy(out=GT.ap, in_=pgt.ap[0:32, :]), reads=pgt.res(), writes=GT.res())
            for nh in range(2):
                pb2 = psb(4 + nh, [512])
                pairs = [(GT.ap, bdn.ap[:, nh * 512:(nh + 1) * 512])]
                for k in range(4):
                    pairs.append((dgt.ap[:, k, :], yt.ap[:, k, nh * 512:(nh + 1) * 512]))
                    pairs.append((dgt.ap[:, 4 + k, :], yt.ap[:, k, nh * 512:(nh + 1) * 512]))
                mm_group(pb2.ap, pairs, reads=GT.res() + bdn.res() + dgt.res() + yt.res(), writes=pb2.res())
                resid_add(t, nh, pb2, gbc[c], tmpY)

    def attn_phase():
        l = 1
        o = 0
        wqkv = sbuf(o, [8, 1536], BF16); o += 24576
        wo = sbuf(o, [8, 1024], BF16); o += 16384
        kT = sbuf(o, [2, 2304], BF16); o += 9216
        V = sbuf(o, [NT, 256], BF16); o += 9216
        qT = sbuf(o, [8, 2048], BF16); o += 32768
        gbc = sbuf(o, [1024]); o += 4096
        qkg = sbuf(o, [256]); o += 1024
        o2 = o
        xn = sbuf(o, [1024], BF16); o += 2048
        hT = [sbuf(o + i * 2048, [8, 128], BF16) for i in range(2)]; o += 4096
        sq = sbuf(o, [10, 128]); o += 5120
        qn = sbuf(o, [10, 128]); o += 5120
        t1 = sbuf(o, [10, 2, 32]); o += 2560
        t2 = sbuf(o, [10, 2, 32]); o += 2560
        qb = sbuf(o, [10, 128], BF16); o += 2560
        cs = sbuf(o, [2, 64]); o += 512
        hs = sbuf(o, [32]); o += 256
        junk = sbuf(o, [1024], BF16); o += 2048
        o = o2
        PT = sbuf(o, [NT, 512], BF16); o += 18432
        OT = sbuf(o, [8, 512], BF16); o += 8192
        rinv = sbuf(o, [512]); o += 2048
        tmpY = sbuf(o, [512]); o += 2048

        load_slab(wqkv, at_w_qkv_d.rearrange("(k p) n -> p k n", p=128))
        load_slab(wo, at_w_o_d.rearrange("(k p) n -> p k n", p=128))
        dma("sp", qkg.ap, at_qk_gain_d.partition_broadcast(128), writes=qkg.res(), key="const")
        build_AB(l, 0, 1)
        build_gbc(l, 2, 0, gbc, psb(7, [512]))
        phase_rstd(list(range(NT)), junk)
        for t in range(NT):
            lat = t < 16
            h = hT[t % 2]
            norm_T(t, xn, psb(7, [8, 128], BF16), h.ap, h.res())
            nbs = [0, 1, 2] if lat else [2]
            pq = [psb(nb, [512]) for nb in range(3)]
            for nb in nbs:
                mm_group(pq[nb].ap, [(h.ap[:, k, :], wqkv.ap[:, k, nb * 512:(nb + 1) * 512]) for k in range(8)],
                         reads=h.res() + wqkv.res(), writes=pq[nb].res())
            act(V.ap[:, t, :], pq[2].ap[:, 256:512], AF.Copy, reads=pq[2].res(), writes=V.sub(t))
            h0 = 0 if lat else 8
            nh_ = 10 - h0
            for nb in nbs:
                lo = nb * 4
                hi = min(lo + 4, 10)
                if lat or nb == 2:
                    a0 = max(lo, h0)
                    act(sq.ap[:, a0:hi, :].rearrange("p a b -> p (a b)"), pq[nb].ap[:, (a0 - lo) * 128:(hi - lo) * 128], AF.Square,
                        reads=pq[nb].res(), writes=sq.sub(a0, hi - a0))
            dve(lambda e, h0=h0: e.tensor_reduce(out=hs.ap[:, h0:10], in_=sq.ap[:, h0:10, :], axis=AX.X, op=ALU.add), reads=sq.res(), writes=hs.res())
            act(hs.ap[:, 10 + h0:20], hs.ap[:, h0:10], AF.Sqrt, reads=hs.res(), writes=hs.res(), bias=EPS, scale=1.0 / 128.0)
            dve(lambda e, h0=h0: e.reciprocal(out=hs.ap[:, 20 + h0:30], in_=hs.ap[:, 10 + h0:20]), reads=hs.res(), writes=hs.res())
            for nb in nbs:
                lo = nb * 4
                hi = min(lo + 4, 10)
                a0 = max(lo, h0)
                dve(lambda e, nb=nb, lo=lo, hi=hi, a0=a0: e.tensor_tensor(
                    out=qn.ap[:, a0:hi, :], in0=pq[nb].ap[:, (a0 - lo) * 128:(hi - lo) * 128].rearrange("p (a b) -> p a b", b=128),
                    in1=hs.ap[:, 20 + a0:20 + hi].unsqueeze(2).to_broadcast([128, hi - a0, 128]), op=ALU.mult),
                    reads=pq[nb].res() + hs.res(), writes=qn.sub(a0, hi - a0))
            if lat:
                dve(lambda e: e.tensor_tensor(out=qn.ap[:, 0:8, :], in0=qn.ap[:, 0:8, :], in1=qkg.ap[:, 0:128].unsqueeze(1).to_broadcast([128, 8, 128]), op=ALU.mult),
                    reads=qn.sub(0, 8) + qkg.res(), writes=qn.sub(0, 8))
            dst_k = qn if lat else qb
            dve(lambda e, dst_k=dst_k: e.tensor_tensor(out=dst_k.ap[:, 8:10, :], in0=qn.ap[:, 8:10, :], in1=qkg.ap[:, 128:256].unsqueeze(1).to_broadcast([128, 2, 128]), op=ALU.mult),
                reads=qn.sub(8, 2) + qkg.res(), writes=dst_k.sub(8, 2))
            if lat:
                dma("sp", cs.ap[:, 0, :], cos_d[t * 128:(t + 1) * 128, :], writes=cs.res(), key="cs")
                dma("sp", cs.ap[:, 1, :], sin_d[t * 128:(t + 1) * 128, :], writes=cs.res(), key="cs")
                q5 = qn.ap.rearrange("p h (a b f) -> p h a b f", a=2, b=2)
                qb5 = qb.ap.rearrange("p h (a b f) -> p h a b f", a=2, b=2)
                x1, x2 = q5[:, :, :, 0, :], q5[:, :, :, 1, :]
                cosb = cs.ap[:, 0, :].rearrange("p (a f) -> p a f", a=2).unsqueeze(1).to_broadcast([128, 10, 2, 32])
                sinb = cs.ap[:, 1, :].rearrange("p (a f) -> p a f", a=2).unsqueeze(1).to_broadcast([128, 10, 2, 32])
                rr = qn.res() + cs.res()
                dve(lambda e: e.tensor_tensor(out=t1.ap, in0=x1, in1=cosb, op=ALU.mult), reads=rr, writes=t1.res())
                dve(lambda e: e.tensor_tensor(out=t2.ap, in0=x2, in1=sinb, op=ALU.mult), reads=rr, writes=t2.res())
                dve(lambda e: e.tensor_tensor(out=qb5[:, :, :, 0, :], in0=t1.ap, in1=t2.ap, op=ALU.subtract), reads=t1.res() + t2.res(), writes=qb.res())
                dve(lambda e: e.tensor_tensor(out=t1.ap, in0=x2, in1=cosb, op=ALU.mult), reads=rr, writes=t1.res())
                dve(lambda e: e.tensor_tensor(out=t2.ap, in0=x1, in1=sinb, op=ALU.mult), reads=rr, writes=t2.res())
                dve(lambda e: e.tensor_tensor(out=qb5[:, :, :, 1, :], in0=t1.ap, in1=t2.ap, op=ALU.add), reads=t1.res() + t2.res(), writes=qb.res())
            ptr = [psb(4 + i, [8, 128], BF16) for i in range(2)]
            hl = list(range(h0, 10))

            def trq(e, hl=hl):
                ins = None
                for hh in hl:
                    ins = e.transpose(out=ptr[hh // 8].ap[:, hh % 8, :], in_=qb.ap[:, hh, :], identity=identb.ap)
                return ins
            P.op("pe", trq, reads=qb.res() + identb.res(), writes=ptr[0].res() + ptr[1].res())
            if lat:
                dve(lambda e, t=t: e.tensor_copy(out=qT.ap[:, :, t * 128:(t + 1) * 128], in_=ptr[0].ap), reads=ptr[0].res(), writes=qT.res())
            dve(lambda e, t=t: e.tensor_copy(out=kT.ap[:, :, t * 128:(t + 1) * 128], in_=ptr[1].ap[:, 0:2, :]), reads=ptr[1].res(), writes=kT.res())

        if stop_after == "attn_proj":
            return
        scale = 128.0 ** -0.5
        for qg in range(4):
            qs = slice(qg * 512, (qg + 1) * 512)
            for hh in range(8):
                kv = hh // 4
                for kt in range(NT):
                    pS = psb(kt % 4, [512])
                    mm_group(pS.ap, [(kT.ap[:, kv, kt * 128:(kt + 1) * 128], qT.ap[:, hh, qs])], reads=kT.res() + qT.res(), writes=pS.res())
                    act(PT.ap[:, kt, :], pS.ap, AF.Exp, reads=pS.res(), writes=PT.sub(kt), scale=scale)
                pO, pR = psb(4, [512]), psb(5, [512])
                for kt in range(NT):
                    def pv_(e, kt=kt, kv=kv):
                        e.matmul(pO.ap, lhsT=V.ap[:, kt, kv * 128:(kv + 1) * 128], rhs=PT.ap[:, kt, :], start=(kt == 0), stop=(kt == NT - 1))
                        return e.matmul(pR.ap, lhsT=onesb.ap, rhs=PT.ap[:, kt, :], start=(kt == 0), stop=(kt == NT - 1))
                    P.op("pe", pv_, reads=V.sub(kt) + PT.sub(kt) + onesb.res(), writes=pO.res() + pR.res())
                dve(lambda e, pR=pR: e.reciprocal(out=rinv.ap, in_=pR.ap), reads=pR.res(), writes=rinv.res())
                dve(lambda e, pO=pO, hh=hh: e.tensor_tensor(out=OT.ap[:, hh, :], in0=pO.ap, in1=rinv.ap, op=ALU.mult), reads=pO.res() + rinv.res(), writes=OT.sub(hh))
            for ti in range(4):
                t = qg * 4 + ti
                for nh in range(2):
                    py = psb(6 + nh, [512])
                    mm_group(py.ap, [(OT.ap[:, hh, ti * 128:(ti + 1) * 128], wo.ap[:, hh, nh * 512:(nh + 1) * 512]) for hh in range(8)],
                             reads=OT.res() + wo.res(), writes=py.res())
                    resid_add(t, nh, py, gbc, tmpY)

    phases = ["gmlp", "moe0", "attn_proj", "attn", "moe1", "all"]
    upto = phases.index(stop_after)
    if upto >= 0:
        gmlp_phase()
    if upto >= 1:
        moe_sparse_phase(0, list(range(NT)))
    if upto >= 2:
        attn_phase()
    if upto >= 4:
        moe_sparse_phase(1, list(range(16)))

    nout = NT if dbg else 16
    for t in range(nout):
        dma("sp", out_d[t * 128:(t + 1) * 128, :], X.ap[:, t, :], reads=X.sub(t), writes=[("out", t)], key="out")
    P.op("sp", None, reads=[("out", t) for t in range(nout)])

    counters = P.finalize()
    sems = {k: es.enter_context(nc.semaphore("s%d" % i)) for i, k in enumerate(counters.keys())}
    with nc.Block() as block:
        P.emit(block, sems)
    es.close()
    return nc, P, counters


def _rope_tables():
    rows = 2048 // 64
    row = np.repeat(np.arange(rows, dtype=np.int32), 64).astype(np.float32)
    col = np.tile(np.arange(64, dtype=np.int32), rows).astype(np.float32)
    inv_freq = (1.0 / (np.float32(10000.0) ** (np.arange(0, 64, 2, dtype=np.float32) / np.float32(64)))).astype(np.float32)
    ang = np.stack([row[:, None] * inv_freq, col[:, None] * inv_freq], axis=1)
    return (np.cos(ang).astype(np.float32).reshape(2048, 64), np.sin(ang).astype(np.float32).reshape(2048, 64))


_CACHE = {}


def make_in_maps(inputs):
    f = lambda a: np.ascontiguousarray(np.asarray(a, dtype=np.float32))
    cos, sin = _rope_tables()
    shared = {
        "ada_w": f(inputs["ada_w"]),
        "ada_b": f(inputs["ada_b"]).reshape(96, 128),
        "norms": f(np.concatenate([inputs["norm_mix"], inputs["norm_ffn"]], axis=0)).reshape(32, 128),
        "gm_w_in": f(inputs["gm_w_in"][0]),
        "gm_b_in": f(inputs["gm_b_in"][0]).reshape(32, 128),
        "gm_v_gain": f(inputs["gm_v_gain"][0]).reshape(16, 128),
        "gm_w_s": f(inputs["gm_w_s"][0]),
        "gm_b_s": f(inputs["gm_b_s"][0]).reshape(1, 1024),
        "gm_w_out": f(inputs["gm_w_out"][0]),
        "at_w_qkv": f(inputs["at_w_qkv"][0]),
        "at_qk_gain": f(np.concatenate([inputs["at_q_gain"][0], inputs["at_k_gain"][0]])).reshape(1, 256),
        "at_w_o": f(inputs["at_w_o"][0]),
        "router_w": f(inputs["moe_router_w"]),
        "router_b": f(inputs["moe_router_b"]).reshape(2, 1, NEXP),
        "WG": np.ascontiguousarray(f(inputs["moe_w_gu"])[:, :, :, 0:1024].reshape(2, NEXP, 8, 128, 1024).transpose(0, 1, 3, 2, 4)).reshape(2 * NEXP * 128, 8192),
        "WU": np.ascontiguousarray(f(inputs["moe_w_gu"])[:, :, :, 1024:2048].reshape(2, NEXP, 8, 128, 1024).transpose(0, 1, 3, 2, 4)).reshape(2 * NEXP * 128, 8192),
        "WD": np.ascontiguousarray(f(inputs["moe_w_down"]).reshape(2, NEXP, 8, 128, 1024).transpose(0, 1, 3, 2, 4)).reshape(2 * NEXP * 128, 8192),
        "BGL": np.ascontiguousarray(f(inputs["moe_b_gu"]).reshape(2, NEXP, 16, 128).transpose(0, 1, 3, 2)).reshape(2 * NEXP * 128, 16),
        "cU0": np.triu(np.ones((128, 128), np.float32), 0),
        "cU1": np.triu(np.ones((128, 128), np.float32), 1),
        "cIota": np.ascontiguousarray(np.broadcast_to(np.arange(128, dtype=np.float32)[None, :], (128, 128))),
        "cPidx": np.ascontiguousarray(np.stack([np.arange(128, dtype=np.float32), np.zeros(128, np.float32)], axis=1)),
        "b_down": f(inputs["moe_b_down"]),
        "ident": np.eye(128, dtype=np.float32),
        "rope_cos": cos,
        "rope_sin": sin,
    }
    x, c, ctx, c_ctx = f(inputs["x"]), f(inputs["c"]), f(inputs["ctx"]), f(inputs["c_ctx"])
    maps = []
    for b in range(8):
        cvec = np.stack([c[b].reshape(8, 128).T, c_ctx.reshape(8, 128).T], axis=-1).reshape(128, 16)
        m = dict(shared)
        m["x"] = x[b]
        m["ctx"] = ctx[b]
        m["cvec"] = np.ascontiguousarray(cvec)
        maps.append(m)
    return maps


def kernel(**inputs):
    if "nc" not in _CACHE:
        _CACHE["nc"] = build_program("all")[0]
    nc = _CACHE["nc"]
    maps = make_in_maps(inputs)
    res = run_bass_kernel_spmd(nc, maps, core_ids=list(range(8)))
    return np.stack([r["out"] for r in res.results], axis=0).astype(np.float32)
```

```python
import contextlib
import numpy as np
import concourse.bass as bass
import concourse.mybir as mybir
from concourse.bass_utils import run_bass_kernel_spmd

F32 = mybir.dt.float32
BF16 = mybir.dt.bfloat16
AF = mybir.ActivationFunctionType
ALU = mybir.AluOpType
AX = mybir.AxisListType

D = 1024
NT = 18
EPS = 1e-6
NEXP = 32
ALPHA = 1.702
LIMIT = 7.0
GR = 256
DBG = {}


class _Op:
    __slots__ = ("eng", "fn", "deps", "dma", "semkey", "signal", "ordinal", "idx")


class Prog:
    ENGS = ("pe", "act", "dve", "pool", "sp")

    def __init__(self):
        self.ops = []
        self.last_w = {}
        self.readers = {}

    def op(self, eng, fn, reads=(), writes=(), dma=None):
        o = _Op()
        o.eng, o.fn, o.dma, o.idx = eng, fn, dma is not None, len(self.ops)
        o.semkey = ("dma", dma) if dma is not None else ("eng", eng)
        o.signal = False
        o.ordinal = 0
        deps = {}
        last_w, readers = self.last_w, self.readers
        for r in reads:
            w = last_w.get(r)
            if w is not None:
                deps[w] = True
        for wr in writes:
            w = last_w.get(wr)
            if w is not None and w not in deps:
                deps[w] = False
            rd = readers.get(wr)
            if rd:
                for i in rd.values():
                    if i not in deps:
                        deps[i] = False
        o.deps = deps
        rk = (eng, o.idx) if o.dma else eng
        for r in reads:
            d = readers.get(r)
            if d is None:
                readers[r] = {rk: o.idx}
            else:
                d[rk] = o.idx
        for wr in writes:
            last_w[wr] = o.idx
            readers[wr] = None
        self.ops.append(o)
        return o

    def finalize(self):
        ops = self.ops
        for o in ops:
            keep = {}
            for d, raw in o.deps.items():
                a = ops[d]
                if (not a.dma) and (not o.dma) and a.eng == o.eng:
                    if a.eng == "pe":
                        continue
                keep[d] = raw
            o.deps = keep
            for d in keep:
                ops[d].signal = True
        counters = {}
        for o in ops:
            if o.signal:
                inc = 16 if o.dma else 1
                counters[o.semkey] = counters.get(o.semkey, 0) + inc
                o.ordinal = counters[o.semkey]
        return counters

    def emit(self, block, sems):
        ops = self.ops
        per_eng = {e: [o for o in ops if o.eng == e] for e in self.ENGS}

        def run(engobj, lst):
            waited = {}
            for o in lst:
                need = {}
                for d in o.deps:
                    a = ops[d]
                    if need.get(a.semkey, 0) < a.ordinal:
                        need[a.semkey] = a.ordinal
                for k, v in need.items():
                    if waited.get(k, 0) < v:
                        engobj.wait_ge(sems[k], v)
                        waited[k] = v
                if o.fn is None:
                    continue
                ins = o.fn(engobj)
                if o.signal:
                    ins.then_inc(sems[o.semkey], 16 if o.dma else 1)

        @block.tensor
        def _(e):
            run(e, per_eng["pe"])

        @block.scalar
        def _(e):
            run(e, per_eng["act"])

        @block.vector
        def _(e):
            run(e, per_eng["dve"])

        @block.gpsimd
        def _(e):
            run(e, per_eng["pool"])

        @block.sync
        def _(e):
            run(e, per_eng["sp"])


class Buf:
    def __init__(self, region, base_ap_f32, off, shape, dt, parts=128):
        self.region, self.off, self.shape, self.dt = region, off, list(shape), dt
        self.esz = 2 if dt == BF16 else 4
        n = 1
        for s in shape:
            n *= s
        self.nbytes = n * self.esz
        assert off % 4 == 0 and self.nbytes % 4 == 0
        flat = base_ap_f32[0:parts, off // 4:(off + self.nbytes) // 4]
        if dt != F32:
            flat = flat.bitcast(dt)
        if len(shape) == 1:
            self.ap = flat
        elif len(shape) == 2:
            self.ap = flat.rearrange("p (a b) -> p a b", a=shape[0])
        elif len(shape) == 3:
            self.ap = flat.rearrange("p (a b c) -> p a b c", a=shape[0], b=shape[1])
        else:
            raise ValueError

    def res(self, lo=0, n=None):
        if n is None:
            n = self.nbytes // self.esz - lo
        b0 = self.off + lo * self.esz
        b1 = b0 + n * self.esz
        gr = 2048 if self.region == "PS" else GR
        return [(self.region, g) for g in range(b0 // gr, (b1 - 1) // gr + 1)]

    def sub(self, i, cnt=1):
        inner = self.nbytes // self.esz // self.shape[0]
        return self.res(i * inner, cnt * inner)


def build_program(stop_after="all"):
    nc = bass.Bass("TRN2", target_bir_lowering=False)
    P = Prog()
    dbg = stop_after != "all"

    def din(name, shape, dt=F32):
        return nc.dram_tensor(name, list(shape), dt, kind="ExternalInput").ap()

    x_d = din("x", [2048, D])
    ctx_d = din("ctx", [256, D])
    cvec_d = din("cvec", [128, 16])
    ada_w_d = din("ada_w", [2, D, 6 * D])
    ada_b_d = din("ada_b", [96, 128])
    norm_d = din("norms", [32, 128])
    gm_w_in_d = din("gm_w_in", [D, 4096])
    gm_b_in_d = din("gm_b_in", [32, 128])
    gm_v_gain_d = din("gm_v_gain", [16, 128])
    gm_w_s_d = din("gm_w_s", [8, 128, 128])
    gm_b_s_d = din("gm_b_s", [1, 1024])
    gm_w_out_d = din("gm_w_out", [2048, D])
    at_w_qkv_d = din("at_w_qkv", [D, 1536])
    at_qk_gain_d = din("at_qk_gain", [1, 256])
    at_w_o_d = din("at_w_o", [D, D])
    r_w_d = din("router_w", [2, D, NEXP])
    r_b_d = din("router_b", [2, 1, NEXP])
    b_dn_d = din("b_down", [2, NEXP, D])
    ident_d = din("ident", [128, 128])
    WG_d = din("WG", [2 * NEXP * 128, 8 * 1024])
    WU_d = din("WU", [2 * NEXP * 128, 8 * 1024])
    WD_d = din("WD", [2 * NEXP * 128, 8 * 1024])
    BGL_d = din("BGL", [2 * NEXP * 128, 16])
    cU0_d = din("cU0", [128, 128])
    cU1_d = din("cU1", [128, 128])
    cIota_d = din("cIota", [128, 128])
    cPidx_d = din("cPidx", [128, 2])
    NBMAX = 50
    xs_d = nc.dram_tensor("xs_scr", [NBMAX * 512, D], BF16, kind="Internal").ap()
    ys_d = nc.dram_tensor("ys_scr", [NBMAX * 512, D], BF16, kind="Internal").ap()
    cos_d = din("rope_cos", [2048, 64])
    sin_d = din("rope_sin", [2048, 64])
    out_d = nc.dram_tensor("out", [NT * 128 if dbg else 2048, D], F32, kind="ExternalOutput").ap()

    es = contextlib.ExitStack()
    XB, SB_, KB = 73728, 133376, 5376
    Xt = es.enter_context(nc.sbuf_tensor("Xreg", [128, XB // 4], F32))
    St = es.enter_context(nc.sbuf_tensor("Sreg", [128, SB_ // 4], F32))
    Kt = es.enter_context(nc.sbuf_tensor("Kreg", [128, KB // 4], F32))
    PSt = es.enter_context(nc.psum_tensor("PSreg", [128, 4096], F32))

    def xb(off, shape, dt=F32):
        return Buf("X", Xt, off, shape, dt)

    def sbuf(off, shape, dt=F32, parts=128):
        assert off + Buf("S", St, off, shape, dt, parts).nbytes <= SB_, (off, shape)
        return Buf("S", St, off, shape, dt, parts)

    koff = [0]

    def kbuf(shape, dt=F32):
        b = Buf("K", Kt, koff[0], shape, dt)
        koff[0] += (b.nbytes + GR - 1) // GR * GR
        assert koff[0] <= KB
        return b

    def psb(bank, shape, dt=F32, off=0):
        return Buf("PS", PSt, bank * 2048 + off, shape, dt)

    X = xb(0, [NT, D])
    identb = kbuf([128], BF16)
    identf = kbuf([128])
    onesb = kbuf([128], BF16)
    MOD = kbuf([2, 48, 2])
    NRM = kbuf([4, 8])
    SIL = kbuf([8, 2])
    AB = kbuf([2, 2, 8])
    ssq = kbuf([NT])
    rstd = kbuf([NT])
    small = kbuf([64])
    BIN = kbuf([32])
    VG = kbuf([16])
    ZB = kbuf([256], BF16)
    SILb = kbuf([8, 2], BF16)

    uniq = [0]

    def dma(eng, out_ap, in_ap, reads=(), writes=(), key="misc"):
        if key in ("const", "constp", "misc"):
            uniq[0] += 1
            key = ("u", uniq[0])
        return P.op(eng, lambda e: e.dma_start(out=out_ap, in_=in_ap), reads=reads, writes=writes, dma=key)

    def mm_group(out_ap, pairs, reads, writes, fp32=False):
        n = len(pairs)

        def fn(e):
            ins = None
            for i, (l, r) in enumerate(pairs):
                ins = e.matmul(out_ap, lhsT=l, rhs=r, start=(i == 0), stop=(i == n - 1))
            return ins
        return P.op("pe", fn, reads=reads, writes=writes)

    def act(out_ap, in_ap, func, reads, writes, **kw):
        return P.op("act", lambda e: e.activation(out=out_ap, in_=in_ap, func=func, **kw), reads=reads, writes=writes)

    def dve(fn, reads, writes):
        return P.op("dve", fn, reads=reads, writes=writes)

    stage = sbuf(SB_ - 1024, [128])
    stage2 = sbuf(SB_ - 512, [128])

    def load_T(src_ap, n, dst_ap, dst_res, ps, stg):
        dma("sp", stg.ap[0:n, :], src_ap, writes=stg.res(), key=("stage", stg.off))
        mm_group(ps.ap[:, 0:n], [(stg.ap[0:n, :], identf.ap[0:n, 0:n])], reads=stg.res() + identf.res(), writes=ps.res(0, n))
        dve(lambda e: e.tensor_copy(out=dst_ap, in_=ps.ap[:, 0:n]), reads=ps.res(0, n), writes=dst_res)

    dma("sp", identf.ap, ident_d, writes=identf.res(), key="const")
    dma("pool", identb.ap, ident_d, writes=identb.res(), key="constp")
    P.op("pool", lambda e: e.memset(onesb.ap, 1.0), writes=onesb.res())
    cv = sbuf(0, [16])
    dma("sp", cv.ap, cvec_d, writes=cv.res(), key="const")
    act(SIL.ap.rearrange("p a b -> p (a b)"), cv.ap, AF.Silu, reads=cv.res(), writes=SIL.res())
    for t in range(NT):
        src = x_d[t * 128:(t + 1) * 128, :] if t < 16 else ctx_d[(t - 16) * 128:(t - 15) * 128, :]
        dma("sp", X.ap[:, t, :], src, writes=X.sub(t), key=("xload", t))

    ps0 = psb(0, [512])
    load_T(norm_d, 32, NRM.ap.rearrange("p a b -> p (a b)"), NRM.res(), ps0, stage)
    dve(lambda e: e.tensor_copy(out=SILb.ap, in_=SIL.ap), reads=SIL.res(), writes=SILb.res())
    pm = psb(2, [2, 48, 2])

    def adaln_dma(l, blk, w, ncol):
        dma("pool", w.ap, ada_w_d[l, :, blk * ncol:(blk + 1) * ncol].rearrange("(k p) n -> p k n", p=128),
            writes=w.res(), key=("adaw", w.off))

    def adaln_mm(l, blk, w, ncol):
        for m in range(ncol // 128):
            j = blk * (ncol // 128) + m
            mm_group(pm.ap[:, l, j, :], [(w.ap[:, k, m * 128:(m + 1) * 128], SILb.ap[:, k, :]) for k in range(8)],
                     reads=w.res() + SILb.res(), writes=pm.res())

    def adaln_fin(l):
        for c in range(2):
            dve(lambda e, c=c: e.tensor_tensor(out=MOD.ap[:, l, :, c], in0=pm.ap[:, l, :, c],
                                               in1=adab.ap[:, l * 48:(l + 1) * 48], op=ALU.add),
                reads=pm.res() + adab.res(), writes=MOD.res())

    def adaln(l, wbufs, ncol):
        for blk in range(6144 // ncol):
            w = wbufs[blk % len(wbufs)]
            adaln_dma(l, blk, w, ncol)
            adaln_mm(l, blk, w, ncol)
        adaln_fin(l)

    adab = kbuf([96])
    load_T(ada_b_d, 96, adab.ap, adab.res(), psb(1, [512]), stage2)
    adaln(0, [sbuf(4096 + i * 8192, [8, 512], BF16) for i in range(2)], 512)

    def build_AB(l, which, nrm_idx):
        n_sh, n_sc = (0, 1) if which == 0 else (3, 4)
        for c in range(2):
            dve(lambda e, c=c: e.scalar_tensor_tensor(out=AB.ap[:, c, 0, :], in0=MOD.ap[:, l, n_sc * 8:(n_sc + 1) * 8, c],
                                                      scalar=1.0, in1=NRM.ap[:, nrm_idx, :], op0=ALU.add, op1=ALU.mult),
                reads=MOD.res() + NRM.res(), writes=AB.res())
            dve(lambda e, c=c: e.tensor_copy(out=AB.ap[:, c, 1, :], in_=MOD.ap[:, l, n_sh * 8:(n_sh + 1) * 8, c]),
                reads=MOD.res(), writes=AB.res())

    def build_gbc(l, n_g, c, dst, ps):
        rep = sbuf(SB_ - 2048, [2, 128])
        for k in range(8):
            r = rep.ap[:, k % 2, :]
            dve(lambda e, k=k, r=r: e.tensor_scalar(out=r, in0=identf.ap, scalar1=0.0, scalar2=MOD.ap[:, l, n_g * 8 + k, c:c + 1],
                                                    op0=ALU.mult, op1=ALU.add),
                reads=MOD.res() + identf.res(), writes=rep.sub(k % 2))
            mm_group(ps.ap[:, (k % 4) * 128:(k % 4 + 1) * 128], [(r, identf.ap)], reads=rep.sub(k % 2) + identf.res(),
                     writes=ps.res((k % 4) * 128, 128))
            if k % 4 == 3:
                h = k // 4
                dve(lambda e, h=h: e.tensor_copy(out=dst.ap[:, h * 512:(h + 1) * 512], in_=ps.ap), reads=ps.res(), writes=dst.res(h * 512, 512))

    def phase_rstd(tiles, junk):
        for t in tiles:
            act(junk.ap, X.ap[:, t, :], AF.Square, reads=X.sub(t), writes=junk.res() + ssq.res(),
                scale=1.0 / 32.0, accum_out=ssq.ap[:, t:t + 1])
        lo, hi = min(tiles), max(tiles) + 1
        act(rstd.ap[:, lo:hi], ssq.ap[:, lo:hi], AF.Sqrt, reads=ssq.res(), writes=rstd.res(), bias=EPS, scale=1.0)
        dve(lambda e: e.reciprocal(out=rstd.ap[:, lo:hi], in_=rstd.ap[:, lo:hi]), reads=rstd.res(), writes=rstd.res())

    def norm_T(t, xn, pst, dst_ap, dst_res):
        c = 0 if t < 16 else 1
        act(xn.ap, X.ap[:, t, :], AF.Copy, reads=X.sub(t) + rstd.res(), writes=xn.res(), scale=rstd.ap[:, t:t + 1])

        def tr(e):
            ins = None
            for k in range(8):
                ins = e.transpose(out=pst.ap[:, k, :], in_=xn.ap[:, k * 128:(k + 1) * 128], identity=identb.ap)
            return ins
        P.op("pe", tr, reads=xn.res() + identb.res(), writes=pst.res())

        def ev(e):
            ins = None
            for k in range(8):
                ins = e.tensor_scalar(out=dst_ap[:, k, :], in0=pst.ap[:, k, :], scalar1=AB.ap[:, c, 0, k:k + 1],
                                      scalar2=AB.ap[:, c, 1, k:k + 1], op0=ALU.mult, op1=ALU.add)
            return ins
        dve(ev, reads=pst.res() + AB.res(), writes=dst_res)

    def resid_add(t, nh, ps, gbc, tmp):
        dve(lambda e: e.tensor_tensor(out=tmp.ap, in0=ps.ap, in1=gbc.ap[:, nh * 512:(nh + 1) * 512], op=ALU.mult),
            reads=ps.res() + gbc.res(nh * 512, 512), writes=tmp.res())
        xs = X.ap[:, t, nh * 512:(nh + 1) * 512]
        xr = X.res(t * D + nh * 512, 512)
        dve(lambda e: e.tensor_tensor(out=xs, in0=xs, in1=tmp.ap, op=ALU.add), reads=xr + tmp.res(), writes=xr)

    slab_ctr = [0]

    def load_slab(dst, src_ap):
        slab_ctr[0] += 1
        dma("pool", dst.ap, src_ap, writes=dst.res(), key=("slab", dst.off))

    def gmlp_phase():
        l = 0
        o = 0
        slabs = []
        for i in range(6):
            slabs.append(sbuf(o, [8, 1024], BF16)); o += 16384
        bsbc = sbuf(o, [8, 128]); o += 4096
        xn = sbuf(o, [1024], BF16); o += 2048
        hT = [sbuf(o, [8, 128], BF16)] * 2; o += 2048
        uvT = sbuf(o, [32, 128], BF16); o += 8192
        vn = sbuf(o, [2048], BF16); o += 4096
        usT = sbuf(o, [16, 128], BF16); o += 4096
        tmpS = sbuf(o, [4, 128])
        tmpY = sbuf(o, [512])
        junk = sbuf(o, [1024], BF16); o += 2048
        gbc1 = sbuf(o, [1024]); o += 4096
        gbc = [gbc1, gbc1]
        wsT = sbuf(o, [8, 128], BF16); o += 2048
        wsl = stage2
        vss = small
        for i in range(4):
            load_slab(slabs[i], gm_w_in_d[:, i * 1024:(i + 1) * 1024].rearrange("(k p) n -> p k n", p=128))
        for i in range(2):
            load_slab(slabs[4 + i], gm_w_out_d[i * 1024:(i + 1) * 1024, :].rearrange("(k p) n -> p k n", p=128))
        dma("sp", bsbc.ap.rearrange("p a b -> p (a b)"), gm_b_s_d.partition_broadcast(128), writes=bsbc.res(), key="const")
        P.op("pool", lambda e: e.memset(ZB.ap, 0.0), writes=ZB.res())
        zsrc = ZB.ap.unsqueeze(1).to_broadcast([128, 64, 256])
        for zi in range(NBMAX * 512 // 2048 + (1 if (NBMAX * 512) % 2048 else 0)):
            r0 = zi * 2048
            r1 = min(r0 + 2048, NBMAX * 512)
            na = (r1 - r0) // 128
            dma("pool", xs_d[r0:r1, :].rearrange("(p a) (b c) -> p (a b) c", p=128, c=256), ZB.ap.unsqueeze(1).to_broadcast([128, na * 4, 256]),
                reads=ZB.res(), writes=[("xsz", zi)], key="zero")
        load_T(gm_b_in_d, 32, BIN.ap, BIN.res(), psb(0, [512]), stage)
        load_T(gm_v_gain_d, 16, VG.ap, VG.res(), psb(1, [512]), stage2)
        for g in range(8):
            dma("sp", wsl.ap, gm_w_s_d[g], writes=wsl.res(), key=("stage", wsl.off))
            pw = psb(g % 2, [128])
            mm_group(pw.ap, [(wsl.ap, identf.ap)], reads=wsl.res() + identf.res(), writes=pw.res())
            dve(lambda e, g=g, pw=pw: e.tensor_copy(out=wsT.ap[:, g, :], in_=pw.ap), reads=pw.res(), writes=wsT.sub(g))
        build_AB(l, 0, 0)
        build_gbc(l, 2, 0, gbc[0], psb(2, [512]))
        phase_rstd(list(range(NT)), junk)
        for t in range(NT):
            c = 0 if t < 16 else 1
            if t == 16:
                build_gbc(l, 2, 1, gbc[1], psb(5, [512]))
            h = hT[t % 2]
            if t == 0:
                norm_T(t, xn, psb(4, [8, 128], BF16), h.ap, h.res())
            for jb in range(8):
                pus = []
                for m in range(4):
                    j = jb * 4 + m
                    pu = psb(jb % 2, [128], F32, off=m * 512)
                    pus.append(pu)
                    sl = slabs[j // 8]
                    c0 = (j % 8) * 128
                    mm_group(pu.ap, [(sl.ap[:, k, c0:c0 + 128], h.ap[:, k, :]) for k in range(8)],
                             reads=sl.res() + h.res(), writes=pu.res())
                for m in range(4):
                    j = jb * 4 + m
                    act(uvT.ap[:, j, :], pus[m].ap, AF.Gelu, reads=pus[m].res() + BIN.res(), writes=uvT.sub(j), bias=BIN.ap[:, j:j + 1], scale=1.0)
            pv = [psb(2 + r, [8, 128], BF16) for r in range(2)]
            for r in range(2):
                def trv(e, r=r):
                    ins = None
                    for i in range(8):
                        ins = e.transpose(out=pv[r].ap[:, i, :], in_=uvT.ap[:, 16 + r * 8 + i, :], identity=identb.ap)
                    return ins
                P.op("pe", trv, reads=uvT.sub(16 + r * 8, 8) + identb.res(), writes=pv[r].res())
                act(junk.ap, pv[r].ap.rearrange("p a b -> p (a b)"), AF.Square, reads=pv[r].res(), writes=junk.res() + vss.res(),
                    accum_out=vss.ap[:, r:r + 1])
            dve(lambda e: e.tensor_tensor(out=vss.ap[:, 2:3], in0=vss.ap[:, 0:1], in1=vss.ap[:, 1:2], op=ALU.add), reads=vss.res(), writes=vss.res())
            act(vss.ap[:, 3:4], vss.ap[:, 2:3], AF.Sqrt, reads=vss.res(), writes=vss.res(), bias=EPS, scale=1.0 / 2048.0)
            dve(lambda e: e.reciprocal(out=vss.ap[:, 4:5], in_=vss.ap[:, 3:4]), reads=vss.res(), writes=vss.res())
            for r in range(2):
                dve(lambda e, r=r: e.tensor_scalar(out=vn.ap[:, r * 1024:(r + 1) * 1024], in0=pv[r].ap.rearrange("p a b -> p (a b)"),
                                                   scalar1=vss.ap[:, 4:5], scalar2=None, op0=ALU.mult),
                    reads=pv[r].res() + vss.res(), writes=vn.res(r * 1024, 1024))
            for q4 in range(4):
                pS = psb(4 + q4 % 2, [4, 128])
                for i in range(4):
                    jj = q4 * 4 + i
                    mm_group(pS.ap[:, i, :], [(vn.ap[:, jj * 128:(jj + 1) * 128], wsT.ap[:, jj // 2, :])],
                             reads=vn.res(jj * 128, 128) + wsT.sub(jj // 2), writes=pS.sub(i))

                def sg(e, q4=q4, pS=pS):
                    ins = None
                    for i in range(4):
                        jj = q4 * 4 + i
                        ins = e.scalar_tensor_tensor(out=tmpS.ap[:, i, :], in0=pS.ap[:, i, :], scalar=VG.ap[:, jj:jj + 1],
                                                     in1=bsbc.ap[:, jj // 2, :], op0=ALU.mult, op1=ALU.add)
                    return ins
                dve(sg, reads=pS.res() + VG.res() + bsbc.res(), writes=tmpS.res())
                dve(lambda e, q4=q4: e.tensor_tensor(out=usT.ap[:, q4 * 4:(q4 + 1) * 4, :], in0=tmpS.ap, in1=uvT.ap[:, q4 * 4:(q4 + 1) * 4, :], op=ALU.mult),
                    reads=tmpS.res() + uvT.sub(q4 * 4, 4), writes=usT.sub(q4 * 4, 4))
            if t + 1 < NT:
                if t + 1 == 16:
                    pass
                norm_T(t + 1, xn, psb(4, [8, 128], BF16), hT[(t + 1) % 2].ap, hT[(t + 1) % 2].res())
            for nh in range(2):
                py = psb(6 + nh, [512])
                mm_group(py.ap, [(usT.ap[:, jj, :], slabs[4 + jj // 8].ap[:, jj % 8, nh * 512:(nh + 1) * 512]) for jj in range(16)],
                         reads=usT.res() + slabs[4].res() + slabs[5].res(), writes=py.res())
                resid_add(t, nh, py, gbc[c], tmpY)

    I32 = mybir.dt.int32
    IOA = bass.IndirectOffsetOnAxis

    def moe_sparse_phase(l, tiles):
        T = len(tiles)
        NB = T + 31
        assert NB <= NBMAX
        o = 0
        ring = []
        for i in range(5):
            ring.append(sbuf(o, [8, 1024], BF16)); o += 16384
        xtok = sbuf(o, [4, 1024], BF16); o += 8192
        fTb = sbuf(o, [8, 512], BF16); o += 8192
        actT = sbuf(o, [8, 512], BF16); o += 8192
        yblk = sbuf(o, [4, 1024], BF16); o += 8192
        tg = sbuf(o, [512]); o += 2048
        tsg = sbuf(o, [512]); o += 2048
        tu = sbuf(o, [512]); o += 2048
        bgb = [sbuf(o + i * 256, [16]) for i in range(2)]; o += 512
        G = sbuf(o, [NT, NEXP]); o += 2304
        M8 = sbuf(o, [NT, 8]); o += 768
        GATE4 = sbuf(o, [NT, 4]); o += 512
        SLOTF = sbuf(o, [NT, 4]); o += 512
        SLOTI = sbuf(o, [NT, 64], I32); o += NT * 256
        WIDX = sbuf(o, [64], I32); o += 256
        assert o <= SB_ - 2048, o
        r = 5 * 16384
        U0 = sbuf(r, [128]); r += 512
        U1 = sbuf(r, [128]); r += 512
        IOTA = sbuf(r, [128]); r += 512
        ONESF = sbuf(r, [128]); r += 512
        PIDX = sbuf(r, [2]); r += 256
        abc = [sbuf(r + i * 4096, [1024]) for i in range(2)]; r += 8192
        bbc = [sbuf(r + i * 4096, [1024]) for i in range(2)]; r += 8192
        ftok = [sbuf(r + i * 2048, [1024], BF16) for i in range(4)]; r += 8192
        tmp32 = sbuf(r, [1024]); r += 4096
        MALL = sbuf(r, [NT, NEXP]); r += 2304
        CP = sbuf(r, [NEXP]); r += 256
        IOTD = sbuf(r, [NEXP]); r += 256
        SL = sbuf(r, [NEXP]); r += 256
        j32 = sbuf(r, [NEXP]); r += 256
        cnt = sbuf(r, [64]); r += 256
        cmp8 = sbuf(r, [8]); r += 256
        diag = sbuf(r, [NEXP]); r += 256
        BSB = sbuf(r, [NEXP]); r += 256
        cmpb = sbuf(r, [64]); r += 256
        bef = sbuf(r, [64]); r += 256
        rbbc = sbuf(r, [NEXP]); r += 256
        LG = sbuf(r, [NT, NEXP]); r += 2304
        assert r <= bgb[0].off + 512, r
        q = 5 * 16384
        q -= 2048; fTt = sbuf(q, [8, 128], BF16)
        q -= 2048; xn = sbuf(q, [1024], BF16)
        q -= 2048; junk = sbuf(q, [1024], BF16)
        q -= 512; RW = sbuf(q, [8, NEXP], BF16)
        q -= 256; ex = sbuf(q, [NEXP])
        q -= 256; sm = sbuf(q, [64])
        wada = []
        for i in range(3):
            q -= 8192; wada.append(sbuf(q, [8, 512], BF16))
        assert q >= 3 * 16384, q
        q = 0
        gbc = [sbuf(q + i * 4096, [1024]) for i in range(2)]; q += 8192
        bdn = sbuf(q, [1024], BF16, parts=32); q += 2048
        Gb = sbuf(q, [NEXP], BF16); q += 256
        GT = sbuf(q, [128], BF16, parts=32); q += 256
        ytok = [sbuf(q + i * 8192, [4, 1024], BF16) for i in range(2)]; q += 16384
        tmpY = sbuf(q, [512]); q += 2048
        dg = [sbuf(q + i * 2048, [8, 128], BF16) for i in range(2)]; q += 4096
        g4 = sbuf(q, [16]); q += 256
        g4b = sbuf(q, [4], BF16); q += 256
        assert q <= 5 * 16384, q
        lbase = float(l * NEXP * 128)

        dma("sp", U0.ap, cU0_d, writes=U0.res(), key="const")
        dma("sp", U1.ap, cU1_d, writes=U1.res(), key="const")
        dma("sp", IOTA.ap, cIota_d, writes=IOTA.res(), key="const")
        dma("sp", PIDX.ap, cPidx_d, writes=PIDX.res(), key="const")
        dve(lambda e: e.memset(ONESF.ap, 1.0), reads=(), writes=ONESF.res())
        dma("pool", RW.ap, r_w_d[l].rearrange("(k p) n -> p k n", p=128), writes=RW.res(), key="const")
        dma("sp", rbbc.ap, r_b_d[l].partition_broadcast(128), writes=rbbc.res(), key="const")
        dve(lambda e: e.tensor_scalar(out=IOTD.ap, in0=IOTA.ap[:, 0:NEXP], scalar1=2.0 ** -20, scalar2=None, op0=ALU.mult), reads=IOTA.res(), writes=IOTD.res())
        dve(lambda e: e.tensor_tensor(out=IOTD.ap, in0=rbbc.ap, in1=IOTD.ap, op=ALU.subtract), reads=rbbc.res() + IOTD.res(), writes=IOTD.res())
        build_AB(l, 1, 2 + l)
        ncls = 2 if l == 0 else 1
        for c in range(ncls):
            for which, dst in ((0, abc[c]), (1, bbc[c])):
                rep = sbuf(SB_ - 2048, [2, 128])
                ps = psb(c * 2 + which, [512])
                for k in range(8):
                    r = rep.ap[:, k % 2, :]
                    dve(lambda e, k=k, r=r, c=c, which=which: e.tensor_scalar(out=r, in0=identf.ap, scalar1=0.0, scalar2=AB.ap[:, c, which, k:k + 1],
                                                                             op0=ALU.mult, op1=ALU.add),
                        reads=AB.res() + identf.res(), writes=rep.sub(k % 2))
                    mm_group(ps.ap[:, (k % 4) * 128:(k % 4 + 1) * 128], [(r, identf.ap)], reads=rep.sub(k % 2) + identf.res(),
                             writes=ps.res((k % 4) * 128, 128))
                    if k % 4 == 3:
                        h = k // 4
                        dve(lambda e, h=h, dst=dst, ps=ps: e.tensor_copy(out=dst.ap[:, h * 512:(h + 1) * 512], in_=ps.ap), reads=ps.res(), writes=dst.res(h * 512, 512))
        phase_rstd(tiles, junk)

        for it1, t in enumerate(tiles):
            c = 0 if t < 16 else 1
            if l == 0:
                if it1 < 12:
                    adaln_dma(1, it1, wada[it1 % 3], 512)
                if 2 <= it1 < 14:
                    adaln_mm(1, it1 - 2, wada[(it1 - 2) % 3], 512)
                if it1 == 14:
                    adaln_fin(1)
            norm_T(t, xn, psb(7, [8, 128], BF16), fTt.ap, fTt.res())
            pl = psb(6, [NEXP])
            mm_group(pl.ap, [(fTt.ap[:, k, :], RW.ap[:, k, :]) for k in range(8)], reads=fTt.res() + RW.res(), writes=pl.res())
            lgt = LG.ap[:, t, :]
            dve(lambda e, pl=pl, lgt=lgt: e.tensor_tensor(out=lgt, in0=pl.ap, in1=IOTD.ap, op=ALU.add), reads=pl.res() + IOTD.res(), writes=LG.sub(t))
            dve(lambda e, t=t, lgt=lgt: e.max(out=M8.ap[:, t, :], in_=lgt), reads=LG.sub(t), writes=M8.sub(t))
            dve(lambda e, t=t: e.tensor_scalar(out=sm.ap[:, 0:1], in0=M8.ap[:, t, 0:1], scalar1=-1.0, scalar2=None, op0=ALU.mult), reads=M8.sub(t), writes=sm.res())
            act(ex.ap, lgt, AF.Exp, reads=LG.sub(t) + sm.res(), writes=ex.res(), bias=sm.ap[:, 0:1], scale=1.0)
            act(sm.ap[:, 4:8], M8.ap[:, t, 0:4], AF.Exp, reads=M8.sub(t) + sm.res(), writes=sm.res(), bias=sm.ap[:, 0:1], scale=1.0)
            dve(lambda e, t=t, lgt=lgt: e.tensor_scalar(out=MALL.ap[:, t, :], in0=lgt, scalar1=M8.ap[:, t, 3:4], scalar2=None, op0=ALU.is_ge),
                reads=LG.sub(t) + M8.sub(t), writes=MALL.sub(t))
            dve(lambda e, t=t: e.tensor_tensor(out=ex.ap, in0=ex.ap, in1=MALL.ap[:, t, :], op=ALU.mult), reads=ex.res() + MALL.sub(t), writes=ex.res())
            dve(lambda e: e.tensor_reduce(out=sm.ap[:, 1:2], in_=ex.ap, axis=AX.X, op=ALU.add), reads=ex.res(), writes=sm.res())
            dve(lambda e: e.reciprocal(out=sm.ap[:, 2:3], in_=sm.ap[:, 1:2]), reads=sm.res(), writes=sm.res())
            dve(lambda e, t=t: e.tensor_scalar(out=G.ap[:, t, :], in0=ex.ap, scalar1=sm.ap[:, 2:3], scalar2=None, op0=ALU.mult),
                reads=ex.res() + sm.res(), writes=G.sub(t))
            dve(lambda e, t=t: e.tensor_scalar(out=GATE4.ap[:, t, :], in0=sm.ap[:, 4:8], scalar1=sm.ap[:, 2:3], scalar2=None, op0=ALU.mult),
                reads=sm.res(), writes=GATE4.sub(t))

        pn = psb(0, [512])
        n_t = len(tiles)

        def cntfn(e):
            ins = None
            for i, t in enumerate(tiles):
                ins = e.matmul(pn.ap[0:32, 0:1], lhsT=MALL.ap[:, t, :], rhs=ONESF.ap[:, 0:1], start=(i == 0), stop=(i == n_t - 1))
            return ins
        P.op("pe", cntfn, reads=MALL.res() + ONESF.res(), writes=pn.res())
        c32 = cnt.ap[0:32, :]
        dve(lambda e: e.tensor_copy(out=c32[:, 0:1], in_=pn.ap[0:32, 0:1]), reads=pn.res(), writes=cnt.res())
        dve(lambda e: e.tensor_scalar(out=cmp8.ap[0:32, :], in0=IOTA.ap[0:32, 0:8], scalar1=512.0, scalar2=c32[:, 0:1], op0=ALU.mult, op1=ALU.is_lt),
            reads=IOTA.res() + cnt.res(), writes=cmp8.res())
        dve(lambda e: e.tensor_reduce(out=c32[:, 1:2], in_=cmp8.ap[0:32, :], axis=AX.X, op=ALU.add), reads=cmp8.res(), writes=cnt.res())
        pb = psb(1, [512])
        mm_group(pb.ap[0:32, 0:1], [(U0.ap[0:32, 0:32], c32[:, 1:2])], reads=U0.res() + cnt.res(), writes=pb.res())
        dve(lambda e: e.tensor_copy(out=c32[:, 2:3], in_=pb.ap[0:32, 0:1]), reads=pb.res(), writes=cnt.res())
        dve(lambda e: e.tensor_tensor(out=c32[:, 3:4], in0=c32[:, 2:3], in1=c32[:, 1:2], op=ALU.subtract), reads=cnt.res(), writes=cnt.res())
        dve(lambda e: e.tensor_scalar(out=c32[:, 4:5], in0=c32[:, 3:4], scalar1=512.0, scalar2=None, op0=ALU.mult), reads=cnt.res(), writes=cnt.res())
        dve(lambda e: e.tensor_scalar(out=diag.ap[0:32, :], in0=identf.ap[0:32, 0:32], scalar1=c32[:, 4:5], scalar2=None, op0=ALU.mult),
            reads=identf.res() + cnt.res(), writes=diag.res())
        pbs = psb(2, [512])
        mm_group(pbs.ap[:, 0:32], [(ONESF.ap[0:32, :], diag.ap[0:32, :])], reads=ONESF.res() + diag.res(), writes=pbs.res())
        dve(lambda e: e.tensor_copy(out=BSB.ap, in_=pbs.ap[:, 0:32]), reads=pbs.res(), writes=BSB.res())
        dve(lambda e: e.tensor_scalar(out=cmpb.ap[0:32, 0:NB], in0=IOTA.ap[0:32, 0:NB], scalar1=c32[:, 2:3], scalar2=None, op0=ALU.is_ge),
            reads=IOTA.res() + cnt.res(), writes=cmpb.res())
        pbe = psb(3, [512])
        mm_group(pbe.ap[:, 0:NB], [(ONESF.ap[0:32, :], cmpb.ap[0:32, 0:NB])], reads=ONESF.res() + cmpb.res(), writes=pbe.res())
        dve(lambda e: e.tensor_scalar(out=bef.ap[:, 0:NB], in0=pbe.ap[:, 0:NB], scalar1=31.0, scalar2=128.0, op0=ALU.min, op1=ALU.mult),
            reads=pbe.res(), writes=bef.res())
        dve(lambda e: e.tensor_scalar(out=bef.ap[:, 0:NB], in0=bef.ap[:, 0:NB], scalar1=PIDX.ap[:, 0:1], scalar2=lbase, op0=ALU.add, op1=ALU.add),
            reads=bef.res() + PIDX.res(), writes=bef.res())
        dve(lambda e: e.tensor_copy(out=WIDX.ap[:, 0:NB], in_=bef.ap[:, 0:NB]), reads=bef.res(), writes=WIDX.res())

        seq = []
        for i in range(NB):
            seq += [(i, "g"), (i, "u"), (i, "d")]
        loaded = [0]

        def ensure(upto):
            while loaded[0] <= upto and loaded[0] < len(seq):
                s_ = loaded[0]
                i, kind = seq[s_]
                dst = ring[s_ % 5]
                src = {"g": WG_d, "u": WU_d, "d": WD_d}[kind]
                P.op("pool", lambda e, dst=dst, src=src, i=i: e.indirect_dma_start(out=dst.ap.rearrange("p k n -> p (k n)"), out_offset=None, in_=src[:, :],
                                                                                 in_offset=IOA(ap=WIDX.ap[:, i:i + 1], axis=0)),
                     reads=WIDX.res(), writes=dst.res(), dma=("slab", dst.off))
                if kind == "g":
                    bb = bgb[i % 2]
                    P.op("pool", lambda e, bb=bb, i=i: e.indirect_dma_start(out=bb.ap, out_offset=None, in_=BGL_d[:, :],
                                                                           in_offset=IOA(ap=WIDX.ap[:, i:i + 1], axis=0)),
                         reads=WIDX.res(), writes=bb.res(), dma=("bgb", i % 2))
                loaded[0] += 1

        XSZ = [("xsz", zi) for zi in range(13)]
        XS_SC = [("xssc", l, it, k) for it in range(len(tiles)) for k in range(4)]
        YS_ALL = [("ys", b) for b in range(NB)]
        dve(lambda e: e.memset(CP.ap, 0.0), reads=(), writes=CP.res())
        for it, t in enumerate(tiles):
            c = 0 if t < 16 else 1
            if it >= 2 and (it - 2) % 3 == 0 and (it - 2) // 3 <= 4:
                ensure((it - 2) // 3)
            pr = psb(4 + it % 2, [512])
            mm_group(pr.ap[:, 0:32], [(U1.ap, MALL.ap[:, t, :]), (ONESF.ap, CP.ap)], reads=U1.res() + MALL.sub(t) + ONESF.res() + CP.res(), writes=pr.res())
            dve(lambda e, pr=pr: e.tensor_tensor(out=SL.ap, in0=pr.ap[:, 0:32], in1=BSB.ap, op=ALU.add), reads=pr.res() + BSB.res(), writes=SL.res())
            dve(lambda e, t=t: e.tensor_tensor(out=CP.ap, in0=CP.ap, in1=MALL.ap[:, t, :], op=ALU.add), reads=CP.res() + MALL.sub(t), writes=CP.res())
            for k in range(4):
                dve(lambda e, t=t, k=k: e.scalar_tensor_tensor(out=j32.ap, in0=LG.ap[:, t, :], scalar=M8.ap[:, t, k:k + 1], in1=SL.ap,
                                                               op0=ALU.is_equal, op1=ALU.mult, accum_out=SLOTF.ap[:, t, k:k + 1]),
                    reads=LG.sub(t) + M8.sub(t) + SL.res(), writes=j32.res() + SLOTF.sub(t))
            dve(lambda e, t=t: e.tensor_copy(out=SLOTI.ap[:, t, 0:4], in_=SLOTF.ap[:, t, :]), reads=SLOTF.sub(t), writes=SLOTI.sub(t))
            ft = ftok[it % 4]
            dve(lambda e, t=t, c=c: e.scalar_tensor_tensor(out=tmp32.ap, in0=X.ap[:, t, :], scalar=rstd.ap[:, t:t + 1], in1=abc[c].ap, op0=ALU.mult, op1=ALU.mult),
                reads=X.sub(t) + rstd.res() + abc[c].res(), writes=tmp32.res())
            dve(lambda e, ft=ft, c=c: e.tensor_tensor(out=ft.ap, in0=tmp32.ap, in1=bbc[c].ap, op=ALU.add), reads=tmp32.res() + bbc[c].res(), writes=ft.res())
            for k in range(4):
                P.op("pool", lambda e, ft=ft, t=t, k=k: e.indirect_dma_start(out=xs_d[0:NB * 512, :], out_offset=IOA(ap=SLOTI.ap[:, t, k:k + 1], axis=0),
                                                                            in_=ft.ap, in_offset=None),
                     reads=ft.res() + SLOTI.sub(t) + XSZ, writes=[("xssc", l, it, k)], dma=("scat", it % 4, k))

        def load_xtok(i):
            dma("sp", xtok.ap, xs_d[i * 512:(i + 1) * 512, :].rearrange("(s p) d -> p s d", p=128), reads=XS_SC, writes=xtok.res(), key="xtok")

        def transposes(i):
            for s4 in range(4):
                pt = psb(6 + s4 % 2, [8, 128], BF16)

                def trb(e, s4=s4, pt=pt):
                    ins = None
                    for k in range(8):
                        ins = e.transpose(out=pt.ap[:, k, :], in_=xtok.ap[:, s4, k * 128:(k + 1) * 128], identity=identb.ap)
                    return ins
                P.op("pe", trb, reads=xtok.sub(s4) + identb.res(), writes=pt.res())
                act(fTb.ap[:, :, s4 * 128:(s4 + 1) * 128], pt.ap, AF.Copy, reads=pt.res(), writes=fTb.res())
            if i + 1 < NB:
                load_xtok(i + 1)

        ensure(4)
        load_xtok(0)
        transposes(0)
        for i in range(NB):
            s0 = 3 * i
            ensure(s0 + 4)
            Wg, Wu, Wd = ring[s0 % 5], ring[(s0 + 1) % 5], ring[(s0 + 2) % 5]
            bb = bgb[i % 2]
            for j in range(8):
                pg = psb(j % 2, [512])
                pu = psb(2 + j % 2, [512])
                mm_group(pg.ap, [(Wg.ap[:, k, j * 128:(j + 1) * 128], fTb.ap[:, k, :]) for k in range(8)], reads=Wg.res() + fTb.res(), writes=pg.res())
                mm_group(pu.ap, [(Wu.ap[:, k, j * 128:(j + 1) * 128], fTb.ap[:, k, :]) for k in range(8)], reads=Wu.res() + fTb.res(), writes=pu.res())
                bg = bb.ap[:, j:j + 1]
                bu = bb.ap[:, 8 + j:9 + j]
                dve(lambda en, pg=pg, bg=bg: en.tensor_scalar(out=tg.ap, in0=pg.ap, scalar1=bg, scalar2=LIMIT, op0=ALU.add, op1=ALU.min),
                    reads=pg.res() + bb.res(), writes=tg.res())
                act(tsg.ap, tg.ap, AF.Silu, reads=tg.res(), writes=tsg.res(), scale=ALPHA)
                dve(lambda en, pu=pu, bu=bu: en.tensor_scalar(out=tu.ap, in0=pu.ap, scalar1=bu, scalar2=LIMIT, op0=ALU.add, op1=ALU.min),
                    reads=pu.res() + bb.res(), writes=tu.res())
                dve(lambda en: en.tensor_scalar(out=tu.ap, in0=tu.ap, scalar1=-LIMIT, scalar2=1.0, op0=ALU.max, op1=ALU.add), reads=tu.res(), writes=tu.res())
                dve(lambda en, j=j: en.scalar_tensor_tensor(out=actT.ap[:, j, :], in0=tsg.ap, scalar=1.0 / ALPHA, in1=tu.ap, op0=ALU.mult, op1=ALU.mult),
                    reads=tsg.res() + tu.res(), writes=actT.sub(j))
            ensure(s0 + 6)
            if i + 1 < NB:
                transposes(i + 1)
            for s4 in range(4):
                for nh in range(2):
                    pd = psb(4 + nh, [512])
                    mm_group(pd.ap, [(actT.ap[:, k, s4 * 128:(s4 + 1) * 128], Wd.ap[:, k, nh * 512:(nh + 1) * 512]) for k in range(8)],
                             reads=actT.res() + Wd.res(), writes=pd.res())
                    act(yblk.ap[:, s4, nh * 512:(nh + 1) * 512], pd.ap, AF.Copy, reads=pd.res(), writes=yblk.res(s4 * 1024 + nh * 512, 512))
            ensure(s0 + 7)
            dma("sp", ys_d[i * 512:(i + 1) * 512, :].rearrange("(s p) d -> p s d", p=128), yblk.ap, reads=yblk.res(), writes=[("ys", i)], key="ysst")

        build_gbc(l, 5, 0, gbc[0], psb(0, [512]))
        if l == 0:
            build_gbc(l, 5, 1, gbc[1], psb(1, [512]))
        dma("pool", bdn.ap, b_dn_d[l], writes=bdn.res(), key="const")
        for it, t in enumerate(tiles):
            c = 0 if t < 16 else 1
            yt = ytok[it % 2]
            for k in range(4):
                P.op("pool", lambda e, yt=yt, t=t, k=k: e.indirect_dma_start(out=yt.ap[:, k, :], out_offset=None, in_=ys_d[0:NB * 512, :],
                                                                            in_offset=IOA(ap=SLOTI.ap[:, t, k:k + 1], axis=0)),
                     reads=YS_ALL + SLOTI.sub(t), writes=yt.sub(k), dma=("gath", it % 2, k))
            dve(lambda e, t=t: e.tensor_copy(out=g4b.ap, in_=GATE4.ap[:, t, :]), reads=GATE4.sub(t), writes=g4b.res())
            dve(lambda e: e.tensor_copy(out=g4.ap[:, 0:4], in_=g4b.ap), reads=g4b.res(), writes=g4.res())
            dve(lambda e, t=t: e.tensor_tensor(out=g4.ap[:, 4:8], in0=GATE4.ap[:, t, :], in1=g4.ap[:, 0:4], op=ALU.subtract),
                reads=GATE4.sub(t) + g4.res(), writes=g4.res())
            dgt = dg[it % 2]

            def mkdiag(e, t=t, dgt=dgt):
                ins = None
                for k in range(4):
                    e.tensor_scalar(out=dgt.ap[:, k, :], in0=identb.ap, scalar1=GATE4.ap[:, t, k:k + 1], scalar2=None, op0=ALU.mult)
                    ins = e.tensor_scalar(out=dgt.ap[:, 4 + k, :], in0=identb.ap, scalar1=g4.ap[:, 4 + k:5 + k], scalar2=None, op0=ALU.mult)
                return ins
            dve(mkdiag, reads=identb.res() + GATE4.sub(t) + g4.res(), writes=dgt.res())
            dve(lambda e, t=t: e.tensor_copy(out=Gb.ap, in_=G.ap[:, t, :]), reads=G.sub(t), writes=Gb.res())
            pgt = psb(6, [128], BF16)
            P.op("pe", lambda e, pgt=pgt: e.transpose(out=pgt.ap[0:32, :], in_=Gb.ap, identity=identb.ap), reads=Gb.res() + identb.res(), writes=pgt.res())
            dve(lambda e, pgt=pgt: e.tensor_copy(out=GT.ap, in_=pgt.ap[0:32, :]), reads=pgt.res(), writes=GT.res())
            for nh in range(2):
                pb2 = psb(4 + nh, [512])
                pairs = [(GT.ap, bdn.ap[:, nh * 512:(nh + 1) * 512])]
                for k in range(4):
                    pairs.append((dgt.ap[:, k, :], yt.ap[:, k, nh * 512:(nh + 1) * 512]))
                    pairs.append((dgt.ap[:, 4 + k, :], yt.ap[:, k, nh * 512:(nh + 1) * 512]))
                mm_group(pb2.ap, pairs, reads=GT.res() + bdn.res() + dgt.res() + yt.res(), writes=pb2.res())
                resid_add(t, nh, pb2, gbc[c], tmpY)

    def attn_phase():
        l = 1
        o = 0
        wqkv = sbuf(o, [8, 1536], BF16); o += 24576
        wo = sbuf(o, [8, 1024], BF16); o += 16384
        kT = sbuf(o, [2, 2304], BF16); o += 9216
        V = sbuf(o, [NT, 256], BF16); o += 9216
        qT = sbuf(o, [8, 2048], BF16); o += 32768
        gbc = sbuf(o, [1024]); o += 4096
        qkg = sbuf(o, [256]); o += 1024
        o2 = o
        xn = sbuf(o, [1024], BF16); o += 2048
        hT = [sbuf(o + i * 2048, [8, 128], BF16) for i in range(2)]; o += 4096
        sq = sbuf(o, [10, 128]); o += 5120
        qn = sbuf(o, [10, 128]); o += 5120
        t1 = sbuf(o, [10, 2, 32]); o += 2560
        t2 = sbuf(o, [10, 2, 32]); o += 2560
        qb = sbuf(o, [10, 128], BF16); o += 2560
        cs = sbuf(o, [2, 64]); o += 512
        hs = sbuf(o, [32]); o += 256
        junk = sbuf(o, [1024], BF16); o += 2048
        o = o2
        PT = sbuf(o, [NT, 512], BF16); o += 18432
        OT = sbuf(o, [8, 512], BF16); o += 8192
        rinv = sbuf(o, [512]); o += 2048
        tmpY = sbuf(o, [512]); o += 2048

        load_slab(wqkv, at_w_qkv_d.rearrange("(k p) n -> p k n", p=128))
        load_slab(wo, at_w_o_d.rearrange("(k p) n -> p k n", p=128))
        dma("sp", qkg.ap, at_qk_gain_d.partition_broadcast(128), writes=qkg.res(), key="const")
        build_AB(l, 0, 1)
        build_gbc(l, 2, 0, gbc, psb(7, [512]))
        phase_rstd(list(range(NT)), junk)
        for t in range(NT):
            lat = t < 16
            h = hT[t % 2]
            norm_T(t, xn, psb(7, [8, 128], BF16), h.ap, h.res())
            nbs = [0, 1, 2] if lat else [2]
            pq = [psb(nb, [512]) for nb in range(3)]
            for nb in nbs:
                mm_group(pq[nb].ap, [(h.ap[:, k, :], wqkv.ap[:, k, nb * 512:(nb + 1) * 512]) for k in range(8)],
                         reads=h.res() + wqkv.res(), writes=pq[nb].res())
            act(V.ap[:, t, :], pq[2].ap[:, 256:512], AF.Copy, reads=pq[2].res(), writes=V.sub(t))
            h0 = 0 if lat else 8
            nh_ = 10 - h0
            for nb in nbs:
                lo = nb * 4
                hi = min(lo + 4, 10)
                if lat or nb == 2:
                    a0 = max(lo, h0)
                    act(sq.ap[:, a0:hi, :].rearrange("p a b -> p (a b)"), pq[nb].ap[:, (a0 - lo) * 128:(hi - lo) * 128], AF.Square,
                        reads=pq[nb].res(), writes=sq.sub(a0, hi - a0))
            dve(lambda e, h0=h0: e.tensor_reduce(out=hs.ap[:, h0:10], in_=sq.ap[:, h0:10, :], axis=AX.X, op=ALU.add), reads=sq.res(), writes=hs.res())
            act(hs.ap[:, 10 + h0:20], hs.ap[:, h0:10], AF.Sqrt, reads=hs.res(), writes=hs.res(), bias=EPS, scale=1.0 / 128.0)
            dve(lambda e, h0=h0: e.reciprocal(out=hs.ap[:, 20 + h0:30], in_=hs.ap[:, 10 + h0:20]), reads=hs.res(), writes=hs.res())
            for nb in nbs:
                lo = nb * 4
                hi = min(lo + 4, 10)
                a0 = max(lo, h0)
                dve(lambda e, nb=nb, lo=lo, hi=hi, a0=a0: e.tensor_tensor(
                    out=qn.ap[:, a0:hi, :], in0=pq[nb].ap[:, (a0 - lo) * 128:(hi - lo) * 128].rearrange("p (a b) -> p a b", b=128),
                    in1=hs.ap[:, 20 + a0:20 + hi].unsqueeze(2).to_broadcast([128, hi - a0, 128]), op=ALU.mult),
                    reads=pq[nb].res() + hs.res(), writes=qn.sub(a0, hi - a0))
            if lat:
                dve(lambda e: e.tensor_tensor(out=qn.ap[:, 0:8, :], in0=qn.ap[:, 0:8, :], in1=qkg.ap[:, 0:128].unsqueeze(1).to_broadcast([128, 8, 128]), op=ALU.mult),
                    reads=qn.sub(0, 8) + qkg.res(), writes=qn.sub(0, 8))
            dst_k = qn if lat else qb
            dve(lambda e, dst_k=dst_k: e.tensor_tensor(out=dst_k.ap[:, 8:10, :], in0=qn.ap[:, 8:10, :], in1=qkg.ap[:, 128:256].unsqueeze(1).to_broadcast([128, 2, 128]), op=ALU.mult),
                reads=qn.sub(8, 2) + qkg.res(), writes=dst_k.sub(8, 2))
            if lat:
                dma("sp", cs.ap[:, 0, :], cos_d[t * 128:(t + 1) * 128, :], writes=cs.res(), key="cs")
                dma("sp", cs.ap[:, 1, :], sin_d[t * 128:(t + 1) * 128, :], writes=cs.res(), key="cs")
                q5 = qn.ap.rearrange("p h (a b f) -> p h a b f", a=2, b=2)
                qb5 = qb.ap.rearrange("p h (a b f) -> p h a b f", a=2, b=2)
                x1, x2 = q5[:, :, :, 0, :], q5[:, :, :, 1, :]
                cosb = cs.ap[:, 0, :].rearrange("p (a f) -> p a f", a=2).unsqueeze(1).to_broadcast([128, 10, 2, 32])
                sinb = cs.ap[:, 1, :].rearrange("p (a f) -> p a f", a=2).unsqueeze(1).to_broadcast([128, 10, 2, 32])
                rr = qn.res() + cs.res()
                dve(lambda e: e.tensor_tensor(out=t1.ap, in0=x1, in1=cosb, op=ALU.mult), reads=rr, writes=t1.res())
                dve(lambda e: e.tensor_tensor(out=t2.ap, in0=x2, in1=sinb, op=ALU.mult), reads=rr, writes=t2.res())
                dve(lambda e: e.tensor_tensor(out=qb5[:, :, :, 0, :], in0=t1.ap, in1=t2.ap, op=ALU.subtract), reads=t1.res() + t2.res(), writes=qb.res())
                dve(lambda e: e.tensor_tensor(out=t1.ap, in0=x2, in1=cosb, op=ALU.mult), reads=rr, writes=t1.res())
                dve(lambda e: e.tensor_tensor(out=t2.ap, in0=x1, in1=sinb, op=ALU.mult), reads=rr, writes=t2.res())
                dve(lambda e: e.tensor_tensor(out=qb5[:, :, :, 1, :], in0=t1.ap, in1=t2.ap, op=ALU.add), reads=t1.res() + t2.res(), writes=qb.res())
            ptr = [psb(4 + i, [8, 128], BF16) for i in range(2)]
            hl = list(range(h0, 10))

            def trq(e, hl=hl):
                ins = None
                for hh in hl:
                    ins = e.transpose(out=ptr[hh // 8].ap[:, hh % 8, :], in_=qb.ap[:, hh, :], identity=identb.ap)
                return ins
            P.op("pe", trq, reads=qb.res() + identb.res(), writes=ptr[0].res() + ptr[1].res())
            if lat:
                dve(lambda e, t=t: e.tensor_copy(out=qT.ap[:, :, t * 128:(t + 1) * 128], in_=ptr[0].ap), reads=ptr[0].res(), writes=qT.res())
            dve(lambda e, t=t: e.tensor_copy(out=kT.ap[:, :, t * 128:(t + 1) * 128], in_=ptr[1].ap[:, 0:2, :]), reads=ptr[1].res(), writes=kT.res())

        if stop_after == "attn_proj":
            return
        scale = 128.0 ** -0.5
        for qg in range(4):
            qs = slice(qg * 512, (qg + 1) * 512)
            for hh in range(8):
                kv = hh // 4
                for kt in range(NT):
                    pS = psb(kt % 4, [512])
                    mm_group(pS.ap, [(kT.ap[:, kv, kt * 128:(kt + 1) * 128], qT.ap[:, hh, qs])], reads=kT.res() + qT.res(), writes=pS.res())
                    act(PT.ap[:, kt, :], pS.ap, AF.Exp, reads=pS.res(), writes=PT.sub(kt), scale=scale)
                pO, pR = psb(4, [512]), psb(5, [512])
                for kt in range(NT):
                    def pv_(e, kt=kt, kv=kv):
                        e.matmul(pO.ap, lhsT=V.ap[:, kt, kv * 128:(kv + 1) * 128], rhs=PT.ap[:, kt, :], start=(kt == 0), stop=(kt == NT - 1))
                        return e.matmul(pR.ap, lhsT=onesb.ap, rhs=PT.ap[:, kt, :], start=(kt == 0), stop=(kt == NT - 1))
                    P.op("pe", pv_, reads=V.sub(kt) + PT.sub(kt) + onesb.res(), writes=pO.res() + pR.res())
                dve(lambda e, pR=pR: e.reciprocal(out=rinv.ap, in_=pR.ap), reads=pR.res(), writes=rinv.res())
                dve(lambda e, pO=pO, hh=hh: e.tensor_tensor(out=OT.ap[:, hh, :], in0=pO.ap, in1=rinv.ap, op=ALU.mult), reads=pO.res() + rinv.res(), writes=OT.sub(hh))
            for ti in range(4):
                t = qg * 4 + ti
                for nh in range(2):
                    py = psb(6 + nh, [512])
                    mm_group(py.ap, [(OT.ap[:, hh, ti * 128:(ti + 1) * 128], wo.ap[:, hh, nh * 512:(nh + 1) * 512]) for hh in range(8)],
                             reads=OT.res() + wo.res(), writes=py.res())
                    resid_add(t, nh, py, gbc, tmpY)

    phases = ["gmlp", "moe0", "attn_proj", "attn", "moe1", "all"]
    upto = phases.index(stop_after)
    if upto >= 0:
        gmlp_phase()
    if upto >= 1:
        moe_sparse_phase(0, list(range(NT)))
    if upto >= 2:
        attn_phase()
    if upto >= 4:
        moe_sparse_phase(1, list(range(16)))

    nout = NT if dbg else 16
    for t in range(nout):
        dma("sp", out_d[t * 128:(t + 1) * 128, :], X.ap[:, t, :], reads=X.sub(t), writes=[("out", t)], key="out")
    P.op("sp", None, reads=[("out", t) for t in range(nout)])

    counters = P.finalize()
    sems = {k: es.enter_context(nc.semaphore("s%d" % i)) for i, k in enumerate(counters.keys())}
    with nc.Block() as block:
        P.emit(block, sems)
    es.close()
    return nc, P, counters


def _rope_tables():
    rows = 2048 // 64
    row = np.repeat(np.arange(rows, dtype=np.int32), 64).astype(np.float32)
    col = np.tile(np.arange(64, dtype=np.int32), rows).astype(np.float32)
    inv_freq = (1.0 / (np.float32(10000.0) ** (np.arange(0, 64, 2, dtype=np.float32) / np.float32(64)))).astype(np.float32)
    ang = np.stack([row[:, None] * inv_freq, col[:, None] * inv_freq], axis=1)
    return (np.cos(ang).astype(np.float32).reshape(2048, 64), np.sin(ang).astype(np.float32).reshape(2048, 64))


_CACHE = {}


def make_in_maps(inputs):
    f = lambda a: np.ascontiguousarray(np.asarray(a, dtype=np.float32))
    cos, sin = _rope_tables()
    shared = {
        "ada_w": f(inputs["ada_w"]),
        "ada_b": f(inputs["ada_b"]).reshape(96, 128),
        "norms": f(np.concatenate([inputs["norm_mix"], inputs["norm_ffn"]], axis=0)).reshape(32, 128),
        "gm_w_in": f(inputs["gm_w_in"][0]),
        "gm_b_in": f(inputs["gm_b_in"][0]).reshape(32, 128),
        "gm_v_gain": f(inputs["gm_v_gain"][0]).reshape(16, 128),
        "gm_w_s": f(inputs["gm_w_s"][0]),
        "gm_b_s": f(inputs["gm_b_s"][0]).reshape(1, 1024),
        "gm_w_out": f(inputs["gm_w_out"][0]),
        "at_w_qkv": f(inputs["at_w_qkv"][0]),
        "at_qk_gain": f(np.concatenate([inputs["at_q_gain"][0], inputs["at_k_gain"][0]])).reshape(1, 256),
        "at_w_o": f(inputs["at_w_o"][0]),
        "router_w": f(inputs["moe_router_w"]),
        "router_b": f(inputs["moe_router_b"]).reshape(2, 1, NEXP),
        "WG": np.ascontiguousarray(f(inputs["moe_w_gu"])[:, :, :, 0:1024].reshape(2, NEXP, 8, 128, 1024).transpose(0, 1, 3, 2, 4)).reshape(2 * NEXP * 128, 8192),
        "WU": np.ascontiguousarray(f(inputs["moe_w_gu"])[:, :, :, 1024:2048].reshape(2, NEXP, 8, 128, 1024).transpose(0, 1, 3, 2, 4)).reshape(2 * NEXP * 128, 8192),
        "WD": np.ascontiguousarray(f(inputs["moe_w_down"]).reshape(2, NEXP, 8, 128, 1024).transpose(0, 1, 3, 2, 4)).reshape(2 * NEXP * 128, 8192),
        "BGL": np.ascontiguousarray(f(inputs["moe_b_gu"]).reshape(2, NEXP, 16, 128).transpose(0, 1, 3, 2)).reshape(2 * NEXP * 128, 16),
        "cU0": np.triu(np.ones((128, 128), np.float32), 0),
        "cU1": np.triu(np.ones((128, 128), np.float32), 1),
        "cIota": np.ascontiguousarray(np.broadcast_to(np.arange(128, dtype=np.float32)[None, :], (128, 128))),
        "cPidx": np.ascontiguousarray(np.stack([np.arange(128, dtype=np.float32), np.zeros(128, np.float32)], axis=1)),
        "b_down": f(inputs["moe_b_down"]),
        "ident": np.eye(128, dtype=np.float32),
        "rope_cos": cos,
        "rope_sin": sin,
    }
    x, c, ctx, c_ctx = f(inputs["x"]), f(inputs["c"]), f(inputs["ctx"]), f(inputs["c_ctx"])
    maps = []
    for b in range(8):
        cvec = np.stack([c[b].reshape(8, 128).T, c_ctx.reshape(8, 128).T], axis=-1).reshape(128, 16)
        m = dict(shared)
        m["x"] = x[b]
        m["ctx"] = ctx[b]
        m["cvec"] = np.ascontiguousarray(cvec)
        maps.append(m)
    return maps


def kernel(**inputs):
    if "nc" not in _CACHE:
        _CACHE["nc"] = build_program("all")[0]
    nc = _CACHE["nc"]
    maps = make_in_maps(inputs)
    res = run_bass_kernel_spmd(nc, maps, core_ids=list(range(8)))
    return np.stack([r["out"] for r in res.results], axis=0).astype(np.float32)
```

```python
import contextlib
import numpy as np
import concourse.bass as bass
import concourse.mybir as mybir
from concourse.bass_utils import run_bass_kernel_spmd

F32 = mybir.dt.float32
BF16 = mybir.dt.bfloat16
AF = mybir.ActivationFunctionType
ALU = mybir.AluOpType
AX = mybir.AxisListType

D = 1024
NT = 18
EPS = 1e-6
NEXP = 32
ALPHA = 1.702
LIMIT = 7.0
GR = 256
DBG = {}


class _Op:
    __slots__ = ("eng", "fn", "deps", "dma", "semkey", "signal", "ordinal", "idx")


class Prog:
    ENGS = ("pe", "act", "dve", "pool", "sp")

    def __init__(self):
        self.ops = []
        self.last_w = {}
        self.readers = {}

    def op(self, eng, fn, reads=(), writes=(), dma=None):
        o = _Op()
        o.eng, o.fn, o.dma, o.idx = eng, fn, dma is not None, len(self.ops)
        o.semkey = ("dma", dma) if dma is not None else ("eng", eng)
        o.signal = False
        o.ordinal = 0
        deps = {}
        last_w, readers = self.last_w, self.readers
        for r in reads:
            w = last_w.get(r)
            if w is not None:
                deps[w] = True
        for wr in writes:
            w = last_w.get(wr)
            if w is not None and w not in deps:
                deps[w] = False
            rd = readers.get(wr)
            if rd:
                for i in rd.values():
                    if i not in deps:
                        deps[i] = False
        o.deps = deps
        rk = (eng, o.idx) if o.dma else eng
        for r in reads:
            d = readers.get(r)
            if d is None:
                readers[r] = {rk: o.idx}
            else:
                d[rk] = o.idx
        for wr in writes:
            last_w[wr] = o.idx
            readers[wr] = None
        self.ops.append(o)
        return o

    def finalize(self):
        ops = self.ops
        for o in ops:
            keep = {}
            for d, raw in o.deps.items():
                a = ops[d]
                if (not a.dma) and (not o.dma) and a.eng == o.eng:
                    if a.eng == "pe":
                        continue
                keep[d] = raw
            o.deps = keep
            for d in keep:
                ops[d].signal = True
        counters = {}
        for o in ops:
            if o.signal:
                inc = 16 if o.dma else 1
                counters[o.semkey] = counters.get(o.semkey, 0) + inc
                o.ordinal = counters[o.semkey]
        return counters

    def emit(self, block, sems):
        ops = self.ops
        per_eng = {e: [o for o in ops if o.eng == e] for e in self.ENGS}

        def run(engobj, lst):
            waited = {}
            for o in lst:
                need = {}
                for d in o.deps:
                    a = ops[d]
                    if need.get(a.semkey, 0) < a.ordinal:
                        need[a.semkey] = a.ordinal
                for k, v in need.items():
                    if waited.get(k, 0) < v:
                        engobj.wait_ge(sems[k], v)
                        waited[k] = v
                if o.fn is None:
                    continue
                ins = o.fn(engobj)
                if o.signal:
                    ins.then_inc(sems[o.semkey], 16 if o.dma else 1)

        @block.tensor
        def _(e):
            run(e, per_eng["pe"])

        @block.scalar
        def _(e):
            run(e, per_eng["act"])

        @block.vector
        def _(e):
            run(e, per_eng["dve"])

        @block.gpsimd
        def _(e):
            run(e, per_eng["pool"])

        @block.sync
        def _(e):
            run(e, per_eng["sp"])


class Buf:
    def __init__(self, region, base_ap_f32, off, shape, dt, parts=128):
        self.region, self.off, self.shape, self.dt = region, off, list(shape), dt
        self.esz = 2 if dt == BF16 else 4
        n = 1
        for s in shape:
            n *= s
        self.nbytes = n * self.esz
        assert off % 4 == 0 and self.nbytes % 4 == 0
        flat = base_ap_f32[0:parts, off // 4:(off + self.nbytes) // 4]
        if dt != F32:
            flat = flat.bitcast(dt)
        if len(shape) == 1:
            self.ap = flat
        elif len(shape) == 2:
            self.ap = flat.rearrange("p (a b) -> p a b", a=shape[0])
        elif len(shape) == 3:
            self.ap = flat.rearrange("p (a b c) -> p a b c", a=shape[0], b=shape[1])
        else:
            raise ValueError

    def res(self, lo=0, n=None):
        if n is None:
            n = self.nbytes // self.esz - lo
        b0 = self.off + lo * self.esz
        b1 = b0 + n * self.esz
        gr = 2048 if self.region == "PS" else GR
        return [(self.region, g) for g in range(b0 // gr, (b1 - 1) // gr + 1)]

    def sub(self, i, cnt=1):
        inner = self.nbytes // self.esz // self.shape[0]
        return self.res(i * inner, cnt * inner)


def build_program(stop_after="all"):
    nc = bass.Bass("TRN2", target_bir_lowering=False)
    P = Prog()
    dbg = stop_after != "all"

    def din(name, shape, dt=F32):
        return nc.dram_tensor(name, list(shape), dt, kind="ExternalInput").ap()

    x_d = din("x", [2048, D])
    ctx_d = din("ctx", [256, D])
    cvec_d = din("cvec", [128, 16])
    ada_w_d = din("ada_w", [2, D, 6 * D])
    ada_b_d = din("ada_b", [96, 128])
    norm_d = din("norms", [32, 128])
    gm_w_in_d = din("gm_w_in", [D, 4096])
    gm_b_in_d = din("gm_b_in", [32, 128])
    gm_v_gain_d = din("gm_v_gain", [16, 128])
    gm_w_s_d = din("gm_w_s", [8, 128, 128])
    gm_b_s_d = din("gm_b_s", [1, 1024])
    gm_w_out_d = din("gm_w_out", [2048, D])
    at_w_qkv_d = din("at_w_qkv", [D, 1536])
    at_qk_gain_d = din("at_qk_gain", [1, 256])
    at_w_o_d = din("at_w_o", [D, D])
    r_w_d = din("router_w", [2, D, NEXP])
    r_b_d = din("router_b", [2, 1, NEXP])
    b_dn_d = din("b_down", [2, NEXP, D])
    ident_d = din("ident", [128, 128])
    WG_d = din("WG", [2 * NEXP * 128, 8 * 1024])
    WU_d = din("WU", [2 * NEXP * 128, 8 * 1024])
    WD_d = din("WD", [2 * NEXP * 128, 8 * 1024])
    BGL_d = din("BGL", [2 * NEXP * 128, 16])
    cU0_d = din("cU0", [128, 128])
    cU1_d = din("cU1", [128, 128])
    cIota_d = din("cIota", [128, 128])
    cPidx_d = din("cPidx", [128, 2])
    NBMAX = 50
    xs_d = nc.dram_tensor("xs_scr", [NBMAX * 512, D], BF16, kind="Internal").ap()
    ys_d = nc.dram_tensor("ys_scr", [NBMAX * 512, D], BF16, kind="Internal").ap()
    cos_d = din("rope_cos", [2048, 64])
    sin_d = din("rope_sin", [2048, 64])
    out_d = nc.dram_tensor("out", [NT * 128 if dbg else 2048, D], F32, kind="ExternalOutput").ap()

    es = contextlib.ExitStack()
    XB, SB_, KB = 73728, 133376, 5376
    Xt = es.enter_context(nc.sbuf_tensor("Xreg", [128, XB // 4], F32))
    St = es.enter_context(nc.sbuf_tensor("Sreg", [128, SB_ // 4], F32))
    Kt = es.enter_context(nc.sbuf_tensor("Kreg", [128, KB // 4], F32))
    PSt = es.enter_context(nc.psum_tensor("PSreg", [128, 4096], F32))

    def xb(off, shape, dt=F32):
        return Buf("X", Xt, off, shape, dt)

    def sbuf(off, shape, dt=F32, parts=128):
        assert off + Buf("S", St, off, shape, dt, parts).nbytes <= SB_, (off, shape)
        return Buf("S", St, off, shape, dt, parts)

    koff = [0]

    def kbuf(shape, dt=F32):
        b = Buf("K", Kt, koff[0], shape, dt)
        koff[0] += (b.nbytes + GR - 1) // GR * GR
        assert koff[0] <= KB
        return b

    def psb(bank, shape, dt=F32, off=0):
        return Buf("PS", PSt, bank * 2048 + off, shape, dt)

    X = xb(0, [NT, D])
    identb = kbuf([128], BF16)
    identf = kbuf([128])
    onesb = kbuf([128], BF16)
    MOD = kbuf([2, 48, 2])
    NRM = kbuf([4, 8])
    SIL = kbuf([8, 2])
    AB = kbuf([2, 2, 8])
    ssq = kbuf([NT])
    rstd = kbuf([NT])
    small = kbuf([64])
    BIN = kbuf([32])
    VG = kbuf([16])
    ZB = kbuf([256], BF16)
    SILb = kbuf([8, 2], BF16)

    uniq = [0]

    def dma(eng, out_ap, in_ap, reads=(), writes=(), key="misc"):
        if key in ("const", "constp", "misc"):
            uniq[0] += 1
            key = ("u", uniq[0])
        return P.op(eng, lambda e: e.dma_start(out=out_ap, in_=in_ap), reads=reads, writes=writes, dma=key)

    def mm_group(out_ap, pairs, reads, writes, fp32=False):
        n = len(pairs)

        def fn(e):
            ins = None
            for i, (l, r) in enumerate(pairs):
                ins = e.matmul(out_ap, lhsT=l, rhs=r, start=(i == 0), stop=(i == n - 1))
            return ins
        return P.op("pe", fn, reads=reads, writes=writes)

    def act(out_ap, in_ap, func, reads, writes, **kw):
        return P.op("act", lambda e: e.activation(out=out_ap, in_=in_ap, func=func, **kw), reads=reads, writes=writes)

    def dve(fn, reads, writes):
        return P.op("dve", fn, reads=reads, writes=writes)

    stage = sbuf(SB_ - 1024, [128])
    stage2 = sbuf(SB_ - 512, [128])

    def load_T(src_ap, n, dst_ap, dst_res, ps, stg):
        dma("sp", stg.ap[0:n, :], src_ap, writes=stg.res(), key=("stage", stg.off))
        mm_group(ps.ap[:, 0:n], [(stg.ap[0:n, :], identf.ap[0:n, 0:n])], reads=stg.res() + identf.res(), writes=ps.res(0, n))
        dve(lambda e: e.tensor_copy(out=dst_ap, in_=ps.ap[:, 0:n]), reads=ps.res(0, n), writes=dst_res)

    dma("sp", identf.ap, ident_d, writes=identf.res(), key="const")
    dma("pool", identb.ap, ident_d, writes=identb.res(), key="constp")
    P.op("pool", lambda e: e.memset(onesb.ap, 1.0), writes=onesb.res())
    cv = sbuf(0, [16])
    dma("sp", cv.ap, cvec_d, writes=cv.res(), key="const")
    act(SIL.ap.rearrange("p a b -> p (a b)"), cv.ap, AF.Silu, reads=cv.res(), writes=SIL.res())
    for t in range(NT):
        src = x_d[t * 128:(t + 1) * 128, :] if t < 16 else ctx_d[(t - 16) * 128:(t - 15) * 128, :]
        dma("sp", X.ap[:, t, :], src, writes=X.sub(t), key=("xload", t))

    ps0 = psb(0, [512])
    load_T(norm_d, 32, NRM.ap.rearrange("p a b -> p (a b)"), NRM.res(), ps0, stage)
    dve(lambda e: e.tensor_copy(out=SILb.ap, in_=SIL.ap), reads=SIL.res(), writes=SILb.res())
    pm = psb(2, [2, 48, 2])

    def adaln_dma(l, blk, w, ncol):
        dma("pool", w.ap, ada_w_d[l, :, blk * ncol:(blk + 1) * ncol].rearrange("(k p) n -> p k n", p=128),
            writes=w.res(), key=("adaw", w.off))

    def adaln_mm(l, blk, w, ncol):
        for m in range(ncol // 128):
            j = blk * (ncol // 128) + m
            mm_group(pm.ap[:, l, j, :], [(w.ap[:, k, m * 128:(m + 1) * 128], SILb.ap[:, k, :]) for k in range(8)],
                     reads=w.res() + SILb.res(), writes=pm.res())

    def adaln_fin(l):
        for c in range(2):
            dve(lambda e, c=c: e.tensor_tensor(out=MOD.ap[:, l, :, c], in0=pm.ap[:, l, :, c],
                                               in1=adab.ap[:, l * 48:(l + 1) * 48], op=ALU.add),
                reads=pm.res() + adab.res(), writes=MOD.res())

    def adaln(l, wbufs, ncol):
        for blk in range(6144 // ncol):
            w = wbufs[blk % len(wbufs)]
            adaln_dma(l, blk, w, ncol)
            adaln_mm(l, blk, w, ncol)
        adaln_fin(l)

    adab = kbuf([96])
    load_T(ada_b_d, 96, adab.ap, adab.res(), psb(1, [512]), stage2)
    adaln(0, [sbuf(4096 + i * 8192, [8, 512], BF16) for i in range(2)], 512)

    def build_AB(l, which, nrm_idx):
        n_sh, n_sc = (0, 1) if which == 0 else (3, 4)
        for c in range(2):
            dve(lambda e, c=c: e.scalar_tensor_tensor(out=AB.ap[:, c, 0, :], in0=MOD.ap[:, l, n_sc * 8:(n_sc + 1) * 8, c],
                                                      scalar=1.0, in1=NRM.ap[:, nrm_idx, :], op0=ALU.add, op1=ALU.mult),
                reads=MOD.res() + NRM.res(), writes=AB.res())
            dve(lambda e, c=c: e.tensor_copy(out=AB.ap[:, c, 1, :], in_=MOD.ap[:, l, n_sh * 8:(n_sh + 1) * 8, c]),
                reads=MOD.res(), writes=AB.res())

    def build_gbc(l, n_g, c, dst, ps):
        rep = sbuf(SB_ - 2048, [2, 128])
        for k in range(8):
            r = rep.ap[:, k % 2, :]
            dve(lambda e, k=k, r=r: e.tensor_scalar(out=r, in0=identf.ap, scalar1=0.0, scalar2=MOD.ap[:, l, n_g * 8 + k, c:c + 1],
                                                    op0=ALU.mult, op1=ALU.add),
                reads=MOD.res() + identf.res(), writes=rep.sub(k % 2))
            mm_group(ps.ap[:, (k % 4) * 128:(k % 4 + 1) * 128], [(r, identf.ap)], reads=rep.sub(k % 2) + identf.res(),
                     writes=ps.res((k % 4) * 128, 128))
            if k % 4 == 3:
                h = k // 4
                dve(lambda e, h=h: e.tensor_copy(out=dst.ap[:, h * 512:(h + 1) * 512], in_=ps.ap), reads=ps.res(), writes=dst.res(h * 512, 512))

    def phase_rstd(tiles, junk):
        for t in tiles:
            act(junk.ap, X.ap[:, t, :], AF.Square, reads=X.sub(t), writes=junk.res() + ssq.res(),
                scale=1.0 / 32.0, accum_out=ssq.ap[:, t:t + 1])
        lo, hi = min(tiles), max(tiles) + 1
        act(rstd.ap[:, lo:hi], ssq.ap[:, lo:hi], AF.Sqrt, reads=ssq.res(), writes=rstd.res(), bias=EPS, scale=1.0)
        dve(lambda e: e.reciprocal(out=rstd.ap[:, lo:hi], in_=rstd.ap[:, lo:hi]), reads=rstd.res(), writes=rstd.res())

    def norm_T(t, xn, pst, dst_ap, dst_res):
        c = 0 if t < 16 else 1
        act(xn.ap, X.ap[:, t, :], AF.Copy, reads=X.sub(t) + rstd.res(), writes=xn.res(), scale=rstd.ap[:, t:t + 1])

        def tr(e):
            ins = None
            for k in range(8):
                ins = e.transpose(out=pst.ap[:, k, :], in_=xn.ap[:, k * 128:(k + 1) * 128], identity=identb.ap)
            return ins
        P.op("pe", tr, reads=xn.res() + identb.res(), writes=pst.res())

        def ev(e):
            ins = None
            for k in range(8):
                ins = e.tensor_scalar(out=dst_ap[:, k, :], in0=pst.ap[:, k, :], scalar1=AB.ap[:, c, 0, k:k + 1],
                                      scalar2=AB.ap[:, c, 1, k:k + 1], op0=ALU.mult, op1=ALU.add)
            return ins
        dve(ev, reads=pst.res() + AB.res(), writes=dst_res)

    def resid_add(t, nh, ps, gbc, tmp):
        dve(lambda e: e.tensor_tensor(out=tmp.ap, in0=ps.ap, in1=gbc.ap[:, nh * 512:(nh + 1) * 512], op=ALU.mult),
            reads=ps.res() + gbc.res(nh * 512, 512), writes=tmp.res())
        xs = X.ap[:, t, nh * 512:(nh + 1) * 512]
        xr = X.res(t * D + nh * 512, 512)
        dve(lambda e: e.tensor_tensor(out=xs, in0=xs, in1=tmp.ap, op=ALU.add), reads=xr + tmp.res(), writes=xr)

    slab_ctr = [0]

    def load_slab(dst, src_ap):
        slab_ctr[0] += 1
        dma("pool", dst.ap, src_ap, writes=dst.res(), key=("slab", dst.off))

    def gmlp_phase():
        l = 0
        o = 0
        slabs = []
        for i in range(6):
            slabs.append(sbuf(o, [8, 1024], BF16)); o += 16384
        bsbc = sbuf(o, [8, 128]); o += 4096
        xn = sbuf(o, [1024], BF16); o += 2048
        hT = [sbuf(o, [8, 128], BF16)] * 2; o += 2048
        uvT = sbuf(o, [32, 128], BF16); o += 8192
        vn = sbuf(o, [2048], BF16); o += 4096
        usT = sbuf(o, [16, 128], BF16); o += 4096
        tmpS = sbuf(o, [4, 128])
        tmpY = sbuf(o, [512])
        junk = sbuf(o, [1024], BF16); o += 2048
        gbc1 = sbuf(o, [1024]); o += 4096
        gbc = [gbc1, gbc1]
        wsT = sbuf(o, [8, 128], BF16); o += 2048
        wsl = stage2
        vss = small
        for i in range(4):
            load_slab(slabs[i], gm_w_in_d[:, i * 1024:(i + 1) * 1024].rearrange("(k p) n -> p k n", p=128))
        for i in range(2):
            load_slab(slabs[4 + i], gm_w_out_d[i * 1024:(i + 1) * 1024, :].rearrange("(k p) n -> p k n", p=128))
        dma("sp", bsbc.ap.rearrange("p a b -> p (a b)"), gm_b_s_d.partition_broadcast(128), writes=bsbc.res(), key="const")
        P.op("pool", lambda e: e.memset(ZB.ap, 0.0), writes=ZB.res())
        zsrc = ZB.ap.unsqueeze(1).to_broadcast([128, 64, 256])
        for zi in range(NBMAX * 512 // 2048 + (1 if (NBMAX * 512) % 2048 else 0)):
            r0 = zi * 2048
            r1 = min(r0 + 2048, NBMAX * 512)
            na = (r1 - r0) // 128
            dma("pool", xs_d[r0:r1, :].rearrange("(p a) (b c) -> p (a b) c", p=128, c=256), ZB.ap.unsqueeze(1).to_broadcast([128, na * 4, 256]),
                reads=ZB.res(), writes=[("xsz", zi)], key="zero")
        load_T(gm_b_in_d, 32, BIN.ap, BIN.res(), psb(0, [512]), stage)
        load_T(gm_v_gain_d, 16, VG.ap, VG.res(), psb(1, [512]), stage2)
        for g in range(8):
            dma("sp", wsl.ap, gm_w_s_d[g], writes=wsl.res(), key=("stage", wsl.off))
            pw = psb(g % 2, [128])
            mm_group(pw.ap, [(wsl.ap, identf.ap)], reads=wsl.res() + identf.res(), writes=pw.res())
            dve(lambda e, g=g, pw=pw: e.tensor_copy(out=wsT.ap[:, g, :], in_=pw.ap), reads=pw.res(), writes=wsT.sub(g))
        build_AB(l, 0, 0)
        build_gbc(l, 2, 0, gbc[0], psb(2, [512]))
        phase_rstd(list(range(NT)), junk)
        for t in range(NT):
            c = 0 if t < 16 else 1
            if t == 16:
                build_gbc(l, 2, 1, gbc[1], psb(5, [512]))
            h = hT[t % 2]
            if t == 0:
                norm_T(t, xn, psb(4, [8, 128], BF16), h.ap, h.res())
            for jb in range(8):
                pus = []
                for m in range(4):
                    j = jb * 4 + m
                    pu = psb(jb % 2, [128], F32, off=m * 512)
                    pus.append(pu)
                    sl = slabs[j // 8]
                    c0 = (j % 8) * 128
                    mm_group(pu.ap, [(sl.ap[:, k, c0:c0 + 128], h.ap[:, k, :]) for k in range(8)],
                             reads=sl.res() + h.res(), writes=pu.res())
                for m in range(4):
                    j = jb * 4 + m
                    act(uvT.ap[:, j, :], pus[m].ap, AF.Gelu, reads=pus[m].res() + BIN.res(), writes=uvT.sub(j), bias=BIN.ap[:, j:j + 1], scale=1.0)
            pv = [psb(2 + r, [8, 128], BF16) for r in range(2)]
            for r in range(2):
                def trv(e, r=r):
                    ins = None
                    for i in range(8):
                        ins = e.transpose(out=pv[r].ap[:, i, :], in_=uvT.ap[:, 16 + r * 8 + i, :], identity=identb.ap)
                    return ins
                P.op("pe", trv, reads=uvT.sub(16 + r * 8, 8) + identb.res(), writes=pv[r].res())
                act(junk.ap, pv[r].ap.rearrange("p a b -> p (a b)"), AF.Square, reads=pv[r].res(), writes=junk.res() + vss.res(),
                    accum_out=vss.ap[:, r:r + 1])
            dve(lambda e: e.tensor_tensor(out=vss.ap[:, 2:3], in0=vss.ap[:, 0:1], in1=vss.ap[:, 1:2], op=ALU.add), reads=vss.res(), writes=vss.res())
            act(vss.ap[:, 3:4], vss.ap[:, 2:3], AF.Sqrt, reads=vss.res(), writes=vss.res(), bias=EPS, scale=1.0 / 2048.0)
            dve(lambda e: e.reciprocal(out=vss.ap[:, 4:5], in_=vss.ap[:, 3:4]), reads=vss.res(), writes=vss.res())
            for r in range(2):
                dve(lambda e, r=r: e.tensor_scalar(out=vn.ap[:, r * 1024:(r + 1) * 1024], in0=pv[r].ap.rearrange("p a b -> p (a b)"),
                                                   scalar1=vss.ap[:, 4:5], scalar2=None, op0=ALU.mult),
                    reads=pv[r].res() + vss.res(), writes=vn.res(r * 1024, 1024))
            for q4 in range(4):
                pS = psb(4 + q4 % 2, [4, 128])
                for i in range(4):
                    jj = q4 * 4 + i
                    mm_group(pS.ap[:, i, :], [(vn.ap[:, jj * 128:(jj + 1) * 128], wsT.ap[:, jj // 2, :])],
                             reads=vn.res(jj * 128, 128) + wsT.sub(jj // 2), writes=pS.sub(i))

                def sg(e, q4=q4, pS=pS):
                    ins = None
                    for i in range(4):
                        jj = q4 * 4 + i
                        ins = e.scalar_tensor_tensor(out=tmpS.ap[:, i, :], in0=pS.ap[:, i, :], scalar=VG.ap[:, jj:jj + 1],
                                                     in1=bsbc.ap[:, jj // 2, :], op0=ALU.mult, op1=ALU.add)
                    return ins
                dve(sg, reads=pS.res() + VG.res() + bsbc.res(), writes=tmpS.res())
                dve(lambda e, q4=q4: e.tensor_tensor(out=usT.ap[:, q4 * 4:(q4 + 1) * 4, :], in0=tmpS.ap, in1=uvT.ap[:, q4 * 4:(q4 + 1) * 4, :], op=ALU.mult),
                    reads=tmpS.res() + uvT.sub(q4 * 4, 4), writes=usT.sub(q4 * 4, 4))
            if t + 1 < NT:
                if t + 1 == 16:
                    pass
                norm_T(t + 1, xn, psb(4, [8, 128], BF16), hT[(t + 1) % 2].ap, hT[(t + 1) % 2].res())
            for nh in range(2):
                py = psb(6 + nh, [512])
                mm_group(py.ap, [(usT.ap[:, jj, :], slabs[4 + jj // 8].ap[:, jj % 8, nh * 512:(nh + 1) * 512]) for jj in range(16)],
                         reads=usT.res() + slabs[4].res() + slabs[5].res(), writes=py.res())
                resid_add(t, nh, py, gbc[c], tmpY)

    I32 = mybir.dt.int32
    IOA = bass.IndirectOffsetOnAxis

    def moe_sparse_phase(l, tiles):
        T = len(tiles)
        NB = T + 31
        assert NB <= NBMAX
        o = 0
        ring = []
        for i in range(5):
            ring.append(sbuf(o, [8, 1024], BF16)); o += 16384
        xtok = sbuf(o, [4, 1024], BF16); o += 8192
        fTb = sbuf(o, [8, 512], BF16); o += 8192
        actT = sbuf(o, [8, 512], BF16); o += 8192
        yblk = sbuf(o, [4, 1024], BF16); o += 8192
        tg = sbuf(o, [512]); o += 2048
        tsg = sbuf(o, [512]); o += 2048
        tu = sbuf(o, [512]); o += 2048
        bgb = [sbuf(o + i * 256, [16]) for i in range(2)]; o += 512
        G = sbuf(o, [NT, NEXP]); o += 2304
        M8 = sbuf(o, [NT, 8]); o += 768
        GATE4 = sbuf(o, [NT, 4]); o += 512
        SLOTF = sbuf(o, [NT, 4]); o += 512
        SLOTI = sbuf(o, [NT, 64], I32); o += NT * 256
        WIDX = sbuf(o, [64], I32); o += 256
        assert o <= SB_ - 2048, o
        r = 5 * 16384
        U0 = sbuf(r, [128]); r += 512
        U1 = sbuf(r, [128]); r += 512
        IOTA = sbuf(r, [128]); r += 512
        ONESF = sbuf(r, [128]); r += 512
        PIDX = sbuf(r, [2]); r += 256
        abc = [sbuf(r + i * 4096, [1024]) for i in range(2)]; r += 8192
        bbc = [sbuf(r + i * 4096, [1024]) for i in range(2)]; r += 8192
        ftok = [sbuf(r + i * 2048, [1024], BF16) for i in range(4)]; r += 8192
        tmp32 = sbuf(r, [1024]); r += 4096
        MALL = sbuf(r, [NT, NEXP]); r += 2304
        CP = sbuf(r, [NEXP]); r += 256
        IOTD = sbuf(r, [NEXP]); r += 256
        SL = sbuf(r, [NEXP]); r += 256
        j32 = sbuf(r, [NEXP]); r += 256
        cnt = sbuf(r, [64]); r += 256
        cmp8 = sbuf(r, [8]); r += 256
        diag = sbuf(r, [NEXP]); r += 256
        BSB = sbuf(r, [NEXP]); r += 256
        cmpb = sbuf(r, [64]); r += 256
        bef = sbuf(r, [64]); r += 256
        rbbc = sbuf(r, [NEXP]); r += 256
        LG = sbuf(r, [NT, NEXP]); r += 2304
        assert r <= bgb[0].off + 512, r
        q = 5 * 16384
        q -= 4096; fTt2 = [sbuf(q + i * 2048, [8, 128], BF16) for i in range(2)]
        q -= 4096; xn2 = [sbuf(q + i * 2048, [1024], BF16) for i in range(2)]
        q -= 2048; junk = sbuf(q, [1024], BF16)
        q -= 512; RW = sbuf(q, [8, NEXP], BF16)
        q -= 256; ex = sbuf(q, [NEXP])
        q -= 256; sm = sbuf(q, [64])
        wada = []
        for i in range(3):
            q -= 8192; wada.append(sbuf(q, [8, 512], BF16))
        assert q >= 2 * 16384, q
        q = 0
        gbc = [sbuf(q + i * 4096, [1024]) for i in range(2)]; q += 8192
        bdn = sbuf(q, [1024], BF16, parts=32); q += 2048
        Gb = sbuf(q, [NEXP], BF16); q += 256
        GT = sbuf(q, [128], BF16, parts=32); q += 256
        ytok = [sbuf(q + i * 8192, [4, 1024], BF16) for i in range(2)]; q += 16384
        tmpY = sbuf(q, [512]); q += 2048
        dg = [sbuf(q + i * 2048, [8, 128], BF16) for i in range(2)]; q += 4096
        g4 = sbuf(q, [16]); q += 256
        g4b = sbuf(q, [4], BF16); q += 256
        assert q <= 5 * 16384, q
        lbase = float(l * NEXP * 128)

        dma("sp", U0.ap, cU0_d, writes=U0.res(), key="const")
        dma("sp", U1.ap, cU1_d, writes=U1.res(), key="const")
        dma("sp", IOTA.ap, cIota_d, writes=IOTA.res(), key="const")
        dma("sp", PIDX.ap, cPidx_d, writes=PIDX.res(), key="const")
        dve(lambda e: e.memset(ONESF.ap, 1.0), reads=(), writes=ONESF.res())
        dma("pool", RW.ap, r_w_d[l].rearrange("(k p) n -> p k n", p=128), writes=RW.res(), key="const")
        dma("sp", rbbc.ap, r_b_d[l].partition_broadcast(128), writes=rbbc.res(), key="const")
        dve(lambda e: e.tensor_scalar(out=IOTD.ap, in0=IOTA.ap[:, 0:NEXP], scalar1=2.0 ** -20, scalar2=None, op0=ALU.mult), reads=IOTA.res(), writes=IOTD.res())
        dve(lambda e: e.tensor_tensor(out=IOTD.ap, in0=rbbc.ap, in1=IOTD.ap, op=ALU.subtract), reads=rbbc.res() + IOTD.res(), writes=IOTD.res())
        build_AB(l, 1, 2 + l)
        ncls = 2 if l == 0 else 1
        for c in range(ncls):
            for which, dst in ((0, abc[c]), (1, bbc[c])):
                rep = sbuf(SB_ - 2048, [2, 128])
                ps = psb(c * 2 + which, [512])
                for k in range(8):
                    r = rep.ap[:, k % 2, :]
                    dve(lambda e, k=k, r=r, c=c, which=which: e.tensor_scalar(out=r, in0=identf.ap, scalar1=0.0, scalar2=AB.ap[:, c, which, k:k + 1],
                                                                             op0=ALU.mult, op1=ALU.add),
                        reads=AB.res() + identf.res(), writes=rep.sub(k % 2))
                    mm_group(ps.ap[:, (k % 4) * 128:(k % 4 + 1) * 128], [(r, identf.ap)], reads=rep.sub(k % 2) + identf.res(),
                             writes=ps.res((k % 4) * 128, 128))
                    if k % 4 == 3:
                        h = k // 4
                        dve(lambda e, h=h, dst=dst, ps=ps: e.tensor_copy(out=dst.ap[:, h * 512:(h + 1) * 512], in_=ps.ap), reads=ps.res(), writes=dst.res(h * 512, 512))
        phase_rstd(tiles, junk)

        for it1, t in enumerate(tiles):
            c = 0 if t < 16 else 1
            if l == 0:
                if it1 < 12:
                    adaln_dma(1, it1, wada[it1 % 3], 512)
                if 2 <= it1 < 14:
                    adaln_mm(1, it1 - 2, wada[(it1 - 2) % 3], 512)
                if it1 == 14:
                    adaln_fin(1)
            if it1 == 0:
                norm_T(t, xn2[0], psb(7, [8, 128], BF16), fTt2[0].ap, fTt2[0].res())
            if it1 + 1 < len(tiles):
                norm_T(tiles[it1 + 1], xn2[(it1 + 1) % 2], psb(7 if (it1 + 1) % 2 == 0 else 5, [8, 128], BF16), fTt2[(it1 + 1) % 2].ap, fTt2[(it1 + 1) % 2].res())
            fTt = fTt2[it1 % 2]
            pl = psb(6, [NEXP])
            mm_group(pl.ap, [(fTt.ap[:, k, :], RW.ap[:, k, :]) for k in range(8)], reads=fTt.res() + RW.res(), writes=pl.res())
            lgt = LG.ap[:, t, :]
            dve(lambda e, pl=pl, lgt=lgt: e.tensor_tensor(out=lgt, in0=pl.ap, in1=IOTD.ap, op=ALU.add), reads=pl.res() + IOTD.res(), writes=LG.sub(t))
            dve(lambda e, t=t, lgt=lgt: e.max(out=M8.ap[:, t, :], in_=lgt), reads=LG.sub(t), writes=M8.sub(t))
            dve(lambda e, t=t: e.tensor_scalar(out=sm.ap[:, 0:1], in0=M8.ap[:, t, 0:1], scalar1=-1.0, scalar2=None, op0=ALU.mult), reads=M8.sub(t), writes=sm.res())
            act(ex.ap, lgt, AF.Exp, reads=LG.sub(t) + sm.res(), writes=ex.res(), bias=sm.ap[:, 0:1], scale=1.0)
            act(sm.ap[:, 4:8], M8.ap[:, t, 0:4], AF.Exp, reads=M8.sub(t) + sm.res(), writes=sm.res(), bias=sm.ap[:, 0:1], scale=1.0)
            dve(lambda e, t=t, lgt=lgt: e.tensor_scalar(out=MALL.ap[:, t, :], in0=lgt, scalar1=M8.ap[:, t, 3:4], scalar2=None, op0=ALU.is_ge),
                reads=LG.sub(t) + M8.sub(t), writes=MALL.sub(t))
            dve(lambda e, t=t: e.tensor_tensor(out=ex.ap, in0=ex.ap, in1=MALL.ap[:, t, :], op=ALU.mult), reads=ex.res() + MALL.sub(t), writes=ex.res())
            dve(lambda e: e.tensor_reduce(out=sm.ap[:, 1:2], in_=ex.ap, axis=AX.X, op=ALU.add), reads=ex.res(), writes=sm.res())
            dve(lambda e: e.reciprocal(out=sm.ap[:, 2:3], in_=sm.ap[:, 1:2]), reads=sm.res(), writes=sm.res())
            dve(lambda e, t=t: e.tensor_scalar(out=G.ap[:, t, :], in0=ex.ap, scalar1=sm.ap[:, 2:3], scalar2=None, op0=ALU.mult),
                reads=ex.res() + sm.res(), writes=G.sub(t))
            dve(lambda e, t=t: e.tensor_scalar(out=GATE4.ap[:, t, :], in0=sm.ap[:, 4:8], scalar1=sm.ap[:, 2:3], scalar2=None, op0=ALU.mult),
                reads=sm.res(), writes=GATE4.sub(t))

        pn = psb(0, [512])
        n_t = len(tiles)

        def cntfn(e):
            ins = None
            for i, t in enumerate(tiles):
                ins = e.matmul(pn.ap[0:32, 0:1], lhsT=MALL.ap[:, t, :], rhs=ONESF.ap[:, 0:1], start=(i == 0), stop=(i == n_t - 1))
            return ins
        P.op("pe", cntfn, reads=MALL.res() + ONESF.res(), writes=pn.res())
        c32 = cnt.ap[0:32, :]
        dve(lambda e: e.tensor_copy(out=c32[:, 0:1], in_=pn.ap[0:32, 0:1]), reads=pn.res(), writes=cnt.res())
        dve(lambda e: e.tensor_scalar(out=cmp8.ap[0:32, :], in0=IOTA.ap[0:32, 0:8], scalar1=512.0, scalar2=c32[:, 0:1], op0=ALU.mult, op1=ALU.is_lt),
            reads=IOTA.res() + cnt.res(), writes=cmp8.res())
        dve(lambda e: e.tensor_reduce(out=c32[:, 1:2], in_=cmp8.ap[0:32, :], axis=AX.X, op=ALU.add), reads=cmp8.res(), writes=cnt.res())
        pb = psb(1, [512])
        mm_group(pb.ap[0:32, 0:1], [(U0.ap[0:32, 0:32], c32[:, 1:2])], reads=U0.res() + cnt.res(), writes=pb.res())
        dve(lambda e: e.tensor_copy(out=c32[:, 2:3], in_=pb.ap[0:32, 0:1]), reads=pb.res(), writes=cnt.res())
        dve(lambda e: e.tensor_tensor(out=c32[:, 3:4], in0=c32[:, 2:3], in1=c32[:, 1:2], op=ALU.subtract), reads=cnt.res(), writes=cnt.res())
        dve(lambda e: e.tensor_scalar(out=c32[:, 4:5], in0=c32[:, 3:4], scalar1=512.0, scalar2=None, op0=ALU.mult), reads=cnt.res(), writes=cnt.res())
        dve(lambda e: e.tensor_scalar(out=diag.ap[0:32, :], in0=identf.ap[0:32, 0:32], scalar1=c32[:, 4:5], scalar2=None, op0=ALU.mult),
            reads=identf.res() + cnt.res(), writes=diag.res())
        pbs = psb(2, [512])
        mm_group(pbs.ap[:, 0:32], [(ONESF.ap[0:32, :], diag.ap[0:32, :])], reads=ONESF.res() + diag.res(), writes=pbs.res())
        dve(lambda e: e.tensor_copy(out=BSB.ap, in_=pbs.ap[:, 0:32]), reads=pbs.res(), writes=BSB.res())
        dve(lambda e: e.tensor_scalar(out=cmpb.ap[0:32, 0:NB], in0=IOTA.ap[0:32, 0:NB], scalar1=c32[:, 2:3], scalar2=None, op0=ALU.is_ge),
            reads=IOTA.res() + cnt.res(), writes=cmpb.res())
        pbe = psb(3, [512])
        mm_group(pbe.ap[:, 0:NB], [(ONESF.ap[0:32, :], cmpb.ap[0:32, 0:NB])], reads=ONESF.res() + cmpb.res(), writes=pbe.res())
        dve(lambda e: e.tensor_scalar(out=bef.ap[:, 0:NB], in0=pbe.ap[:, 0:NB], scalar1=31.0, scalar2=128.0, op0=ALU.min, op1=ALU.mult),
            reads=pbe.res(), writes=bef.res())
        dve(lambda e: e.tensor_scalar(out=bef.ap[:, 0:NB], in0=bef.ap[:, 0:NB], scalar1=PIDX.ap[:, 0:1], scalar2=lbase, op0=ALU.add, op1=ALU.add),
            reads=bef.res() + PIDX.res(), writes=bef.res())
        dve(lambda e: e.tensor_copy(out=WIDX.ap[:, 0:NB], in_=bef.ap[:, 0:NB]), reads=bef.res(), writes=WIDX.res())

        seq = []
        for i in range(NB):
            seq += [(i, "g"), (i, "u"), (i, "d")]
        loaded = [0]

        def ensure(upto):
            while loaded[0] <= upto and loaded[0] < len(seq):
                s_ = loaded[0]
                i, kind = seq[s_]
                dst = ring[s_ % 5]
                src = {"g": WG_d, "u": WU_d, "d": WD_d}[kind]
                P.op("pool", lambda e, dst=dst, src=src, i=i: e.indirect_dma_start(out=dst.ap.rearrange("p k n -> p (k n)"), out_offset=None, in_=src[:, :],
                                                                                 in_offset=IOA(ap=WIDX.ap[:, i:i + 1], axis=0)),
                     reads=WIDX.res(), writes=dst.res(), dma=("slab", dst.off))
                if kind == "g":
                    bb = bgb[i % 2]
                    P.op("pool", lambda e, bb=bb, i=i: e.indirect_dma_start(out=bb.ap, out_offset=None, in_=BGL_d[:, :],
                                                                           in_offset=IOA(ap=WIDX.ap[:, i:i + 1], axis=0)),
                         reads=WIDX.res(), writes=bb.res(), dma=("bgb", i % 2))
                loaded[0] += 1

        XSZ = [("xsz", zi) for zi in range(13)]
        XS_SC = [("xssc", l, it, k) for it in range(len(tiles)) for k in range(4)]
        YS_ALL = [("ys", b) for b in range(NB)]
        dve(lambda e: e.memset(CP.ap, 0.0), reads=(), writes=CP.res())
        for it, t in enumerate(tiles):
            c = 0 if t < 16 else 1
            if it >= 2 and (it - 2) % 3 == 0 and (it - 2) // 3 <= 4:
                ensure((it - 2) // 3)
            pr = psb(4 + it % 2, [512])
            mm_group(pr.ap[:, 0:32], [(U1.ap, MALL.ap[:, t, :]), (ONESF.ap, CP.ap)], reads=U1.res() + MALL.sub(t) + ONESF.res() + CP.res(), writes=pr.res())
            dve(lambda e, pr=pr: e.tensor_tensor(out=SL.ap, in0=pr.ap[:, 0:32], in1=BSB.ap, op=ALU.add), reads=pr.res() + BSB.res(), writes=SL.res())
            dve(lambda e, t=t: e.tensor_tensor(out=CP.ap, in0=CP.ap, in1=MALL.ap[:, t, :], op=ALU.add), reads=CP.res() + MALL.sub(t), writes=CP.res())
            for k in range(4):
                dve(lambda e, t=t, k=k: e.scalar_tensor_tensor(out=j32.ap, in0=LG.ap[:, t, :], scalar=M8.ap[:, t, k:k + 1], in1=SL.ap,
                                                               op0=ALU.is_equal, op1=ALU.mult, accum_out=SLOTF.ap[:, t, k:k + 1]),
                    reads=LG.sub(t) + M8.sub(t) + SL.res(), writes=j32.res() + SLOTF.sub(t))
            dve(lambda e, t=t: e.tensor_copy(out=SLOTI.ap[:, t, 0:4], in_=SLOTF.ap[:, t, :]), reads=SLOTF.sub(t), writes=SLOTI.sub(t))
            ft = ftok[it % 4]
            dve(lambda e, t=t, c=c: e.scalar_tensor_tensor(out=tmp32.ap, in0=X.ap[:, t, :], scalar=rstd.ap[:, t:t + 1], in1=abc[c].ap, op0=ALU.mult, op1=ALU.mult),
                reads=X.sub(t) + rstd.res() + abc[c].res(), writes=tmp32.res())
            dve(lambda e, ft=ft, c=c: e.tensor_tensor(out=ft.ap, in0=tmp32.ap, in1=bbc[c].ap, op=ALU.add), reads=tmp32.res() + bbc[c].res(), writes=ft.res())
            for k in range(4):
                P.op("pool", lambda e, ft=ft, t=t, k=k: e.indirect_dma_start(out=xs_d[0:NB * 512, :], out_offset=IOA(ap=SLOTI.ap[:, t, k:k + 1], axis=0),
                                                                            in_=ft.ap, in_offset=None),
                     reads=ft.res() + SLOTI.sub(t) + XSZ, writes=[("xssc", l, it, k)], dma=("scat", it % 4, k))

        def load_xtok(i):
            dma("sp", xtok.ap, xs_d[i * 512:(i + 1) * 512, :].rearrange("(s p) d -> p s d", p=128), reads=XS_SC, writes=xtok.res(), key="xtok")

        def transposes(i):
            for s4 in range(4):
                pt = psb(6 + s4 % 2, [8, 128], BF16)

                def trb(e, s4=s4, pt=pt):
                    ins = None
                    for k in range(8):
                        ins = e.transpose(out=pt.ap[:, k, :], in_=xtok.ap[:, s4, k * 128:(k + 1) * 128], identity=identb.ap)
                    return ins
                P.op("pe", trb, reads=xtok.sub(s4) + identb.res(), writes=pt.res())
                act(fTb.ap[:, :, s4 * 128:(s4 + 1) * 128], pt.ap, AF.Copy, reads=pt.res(), writes=fTb.res())
            if i + 1 < NB:
                load_xtok(i + 1)

        ensure(4)
        load_xtok(0)
        transposes(0)
        for i in range(NB):
            s0 = 3 * i
            ensure(s0 + 4)
            Wg, Wu, Wd = ring[s0 % 5], ring[(s0 + 1) % 5], ring[(s0 + 2) % 5]
            bb = bgb[i % 2]
            for j in range(8):
                pg = psb(j % 2, [512])
                pu = psb(2 + j % 2, [512])
                mm_group(pg.ap, [(Wg.ap[:, k, j * 128:(j + 1) * 128], fTb.ap[:, k, :]) for k in range(8)], reads=Wg.res() + fTb.res(), writes=pg.res())
                mm_group(pu.ap, [(Wu.ap[:, k, j * 128:(j + 1) * 128], fTb.ap[:, k, :]) for k in range(8)], reads=Wu.res() + fTb.res(), writes=pu.res())
                bg = bb.ap[:, j:j + 1]
                bu = bb.ap[:, 8 + j:9 + j]
                dve(lambda en, pg=pg, bg=bg: en.tensor_scalar(out=tg.ap, in0=pg.ap, scalar1=bg, scalar2=LIMIT, op0=ALU.add, op1=ALU.min),
                    reads=pg.res() + bb.res(), writes=tg.res())
                act(tsg.ap, tg.ap, AF.Silu, reads=tg.res(), writes=tsg.res(), scale=ALPHA)
                dve(lambda en, pu=pu, bu=bu: en.tensor_scalar(out=tu.ap, in0=pu.ap, scalar1=bu, scalar2=LIMIT, op0=ALU.add, op1=ALU.min),
                    reads=pu.res() + bb.res(), writes=tu.res())
                dve(lambda en: en.tensor_scalar(out=tu.ap, in0=tu.ap, scalar1=-LIMIT, scalar2=1.0, op0=ALU.max, op1=ALU.add), reads=tu.res(), writes=tu.res())
                dve(lambda en, j=j: en.scalar_tensor_tensor(out=actT.ap[:, j, :], in0=tsg.ap, scalar=1.0 / ALPHA, in1=tu.ap, op0=ALU.mult, op1=ALU.mult),
                    reads=tsg.res() + tu.res(), writes=actT.sub(j))
            ensure(s0 + 6)
            if i + 1 < NB:
                transposes(i + 1)
            for s4 in range(4):
                for nh in range(2):
                    pd = psb(4 + nh, [512])
                    mm_group(pd.ap, [(actT.ap[:, k, s4 * 128:(s4 + 1) * 128], Wd.ap[:, k, nh * 512:(nh + 1) * 512]) for k in range(8)],
                             reads=actT.res() + Wd.res(), writes=pd.res())
                    act(yblk.ap[:, s4, nh * 512:(nh + 1) * 512], pd.ap, AF.Copy, reads=pd.res(), writes=yblk.res(s4 * 1024 + nh * 512, 512))
            ensure(s0 + 7)
            dma("sp", ys_d[i * 512:(i + 1) * 512, :].rearrange("(s p) d -> p s d", p=128), yblk.ap, reads=yblk.res(), writes=[("ys", i)], key="ysst")

        build_gbc(l, 5, 0, gbc[0], psb(0, [512]))
        if l == 0:
            build_gbc(l, 5, 1, gbc[1], psb(1, [512]))
        dma("pool", bdn.ap, b_dn_d[l], writes=bdn.res(), key="const")
        for it, t in enumerate(tiles):
            c = 0 if t < 16 else 1
            yt = ytok[it % 2]
            for k in range(4):
                P.op("pool", lambda e, yt=yt, t=t, k=k: e.indirect_dma_start(out=yt.ap[:, k, :], out_offset=None, in_=ys_d[0:NB * 512, :],
                                                                            in_offset=IOA(ap=SLOTI.ap[:, t, k:k + 1], axis=0)),
                     reads=YS_ALL + SLOTI.sub(t), writes=yt.sub(k), dma=("gath", it % 2, k))
            dve(lambda e, t=t: e.tensor_copy(out=g4b.ap, in_=GATE4.ap[:, t, :]), reads=GATE4.sub(t), writes=g4b.res())
            dve(lambda e: e.tensor_copy(out=g4.ap[:, 0:4], in_=g4b.ap), reads=g4b.res(), writes=g4.res())
            dve(lambda e, t=t: e.tensor_tensor(out=g4.ap[:, 4:8], in0=GATE4.ap[:, t, :], in1=g4.ap[:, 0:4], op=ALU.subtract),
                reads=GATE4.sub(t) + g4.res(), writes=g4.res())
            dgt = dg[it % 2]

            def mkdiag(e, t=t, dgt=dgt):
                ins = None
                for k in range(4):
                    e.tensor_scalar(out=dgt.ap[:, k, :], in0=identb.ap, scalar1=GATE4.ap[:, t, k:k + 1], scalar2=None, op0=ALU.mult)
                    ins = e.tensor_scalar(out=dgt.ap[:, 4 + k, :], in0=identb.ap, scalar1=g4.ap[:, 4 + k:5 + k], scalar2=None, op0=ALU.mult)
                return ins
            dve(mkdiag, reads=identb.res() + GATE4.sub(t) + g4.res(), writes=dgt.res())
            dve(lambda e, t=t: e.tensor_copy(out=Gb.ap, in_=G.ap[:, t, :]), reads=G.sub(t), writes=Gb.res())
            pgt = psb(6, [128], BF16)
            P.op("pe", lambda e, pgt=pgt: e.transpose(out=pgt.ap[0:32, :], in_=Gb.ap, identity=identb.ap), reads=Gb.res() + identb.res(), writes=pgt.res())
            dve(lambda e, pgt=pgt: e.tensor_copy(out=GT.ap, in_=pgt.ap[0:32, :]), reads=pgt.res(), writes=GT.res())
            for nh in range(2):
                pb2 = psb(4 + nh, [512])
                pairs = [(GT.ap, bdn.ap[:, nh * 512:(nh + 1) * 512])]
                for k in range(4):
                    pairs.append((dgt.ap[:, k, :], yt.ap[:, k, nh * 512:(nh + 1) * 512]))
                    pairs.append((dgt.ap[:, 4 + k, :], yt.ap[:, k, nh * 512:(nh + 1) * 512]))
                mm_group(pb2.ap, pairs, reads=GT.res() + bdn.res() + dgt.res() + yt.res(), writes=pb2.res())
                resid_add(t, nh, pb2, gbc[c], tmpY)

    def attn_phase():
        l = 1
        o = 0
        wqkv = sbuf(o, [8, 1536], BF16); o += 24576
        wo = sbuf(o, [8, 1024], BF16); o += 16384
        kT = sbuf(o, [2, 2304], BF16); o += 9216
        V = sbuf(o, [NT, 256], BF16); o += 9216
        qT = sbuf(o, [8, 2048], BF16); o += 32768
        gbc = sbuf(o, [1024]); o += 4096
        qkg = sbuf(o, [256]); o += 1024
        o2 = o
        xn = sbuf(o, [1024], BF16); o += 2048
        hT = [sbuf(o + i * 2048, [8, 128], BF16) for i in range(2)]; o += 4096
        sq = sbuf(o, [10, 128]); o += 5120
        qn = sbuf(o, [10, 128]); o += 5120
        t1 = sbuf(o, [10, 2, 32]); o += 2560
        t2 = sbuf(o, [10, 2, 32]); o += 2560
        qb = sbuf(o, [10, 128], BF16); o += 2560
        cs = sbuf(o, [2, 64]); o += 512
        hs = sbuf(o, [32]); o += 256
        junk = sbuf(o, [1024], BF16); o += 2048
        o = o2
        PT = sbuf(o, [NT, 512], BF16); o += 18432
        OT = sbuf(o, [8, 512], BF16); o += 8192
        rinv = sbuf(o, [512]); o += 2048
        tmpY = sbuf(o, [512]); o += 2048

        load_slab(wqkv, at_w_qkv_d.rearrange("(k p) n -> p k n", p=128))
        load_slab(wo, at_w_o_d.rearrange("(k p) n -> p k n", p=128))
        dma("sp", qkg.ap, at_qk_gain_d.partition_broadcast(128), writes=qkg.res(), key="const")
        build_AB(l, 0, 1)
        build_gbc(l, 2, 0, gbc, psb(7, [512]))
        phase_rstd(list(range(NT)), junk)
        PQ = [[psb(b, [512]) for b in (0, 1, 2)], [psb(b, [512]) for b in (3, 5, 6)]]

        def qkv_mm(t):
            h = hT[t % 2]
            norm_T(t, xn, psb(7, [8, 128], BF16), h.ap, h.res())
            pq_ = PQ[t % 2]
            for nb in ([0, 1, 2] if t < 16 else [2]):
                mm_group(pq_[nb].ap, [(h.ap[:, k, :], wqkv.ap[:, k, nb * 512:(nb + 1) * 512]) for k in range(8)],
                         reads=h.res() + wqkv.res(), writes=pq_[nb].res())

        qkv_mm(0)
        for t in range(NT):
            lat = t < 16
            if t + 1 < NT:
                qkv_mm(t + 1)
            nbs = [0, 1, 2] if lat else [2]
            pq = PQ[t % 2]
            act(V.ap[:, t, :], pq[2].ap[:, 256:512], AF.Copy, reads=pq[2].res(), writes=V.sub(t))
            h0 = 0 if lat else 8
            nh_ = 10 - h0
            for nb in nbs:
                lo = nb * 4
                hi = min(lo + 4, 10)
                if lat or nb == 2:
                    a0 = max(lo, h0)
                    act(sq.ap[:, a0:hi, :].rearrange("p a b -> p (a b)"), pq[nb].ap[:, (a0 - lo) * 128:(hi - lo) * 128], AF.Square,
                        reads=pq[nb].res(), writes=sq.sub(a0, hi - a0))
            dve(lambda e, h0=h0: e.tensor_reduce(out=hs.ap[:, h0:10], in_=sq.ap[:, h0:10, :], axis=AX.X, op=ALU.add), reads=sq.res(), writes=hs.res())
            act(hs.ap[:, 10 + h0:20], hs.ap[:, h0:10], AF.Sqrt, reads=hs.res(), writes=hs.res(), bias=EPS, scale=1.0 / 128.0)
            dve(lambda e, h0=h0: e.reciprocal(out=hs.ap[:, 20 + h0:30], in_=hs.ap[:, 10 + h0:20]), reads=hs.res(), writes=hs.res())
            for nb in nbs:
                lo = nb * 4
                hi = min(lo + 4, 10)
                a0 = max(lo, h0)
                dve(lambda e, nb=nb, lo=lo, hi=hi, a0=a0, pq=pq: e.tensor_tensor(
                    out=qn.ap[:, a0:hi, :], in0=pq[nb].ap[:, (a0 - lo) * 128:(hi - lo) * 128].rearrange("p (a b) -> p a b", b=128),
                    in1=hs.ap[:, 20 + a0:20 + hi].unsqueeze(2).to_broadcast([128, hi - a0, 128]), op=ALU.mult),
                    reads=pq[nb].res() + hs.res(), writes=qn.sub(a0, hi - a0))
            if lat:
                dve(lambda e: e.tensor_tensor(out=qn.ap[:, 0:8, :], in0=qn.ap[:, 0:8, :], in1=qkg.ap[:, 0:128].unsqueeze(1).to_broadcast([128, 8, 128]), op=ALU.mult),
                    reads=qn.sub(0, 8) + qkg.res(), writes=qn.sub(0, 8))
            dst_k = qn if lat else qb
            dve(lambda e, dst_k=dst_k: e.tensor_tensor(out=dst_k.ap[:, 8:10, :], in0=qn.ap[:, 8:10, :], in1=qkg.ap[:, 128:256].unsqueeze(1).to_broadcast([128, 2, 128]), op=ALU.mult),
                reads=qn.sub(8, 2) + qkg.res(), writes=dst_k.sub(8, 2))
            if lat:
                dma("sp", cs.ap[:, 0, :], cos_d[t * 128:(t + 1) * 128, :], writes=cs.res(), key="cs")
                dma("sp", cs.ap[:, 1, :], sin_d[t * 128:(t + 1) * 128, :], writes=cs.res(), key="cs")
                q5 = qn.ap.rearrange("p h (a b f) -> p h a b f", a=2, b=2)
                qb5 = qb.ap.rearrange("p h (a b f) -> p h a b f", a=2, b=2)
                x1, x2 = q5[:, :, :, 0, :], q5[:, :, :, 1, :]
                cosb = cs.ap[:, 0, :].rearrange("p (a f) -> p a f", a=2).unsqueeze(1).to_broadcast([128, 10, 2, 32])
                sinb = cs.ap[:, 1, :].rearrange("p (a f) -> p a f", a=2).unsqueeze(1).to_broadcast([128, 10, 2, 32])
                rr = qn.res() + cs.res()
                dve(lambda e: e.tensor_tensor(out=t1.ap, in0=x1, in1=cosb, op=ALU.mult), reads=rr, writes=t1.res())
                dve(lambda e: e.tensor_tensor(out=t2.ap, in0=x2, in1=sinb, op=ALU.mult), reads=rr, writes=t2.res())
                dve(lambda e: e.tensor_tensor(out=qb5[:, :, :, 0, :], in0=t1.ap, in1=t2.ap, op=ALU.subtract), reads=t1.res() + t2.res(), writes=qb.res())
                dve(lambda e: e.tensor_tensor(out=t1.ap, in0=x2, in1=cosb, op=ALU.mult), reads=rr, writes=t1.res())
                dve(lambda e: e.tensor_tensor(out=t2.ap, in0=x1, in1=sinb, op=ALU.mult), reads=rr, writes=t2.res())
                dve(lambda e: e.tensor_tensor(out=qb5[:, :, :, 1, :], in0=t1.ap, in1=t2.ap, op=ALU.add), reads=t1.res() + t2.res(), writes=qb.res())
            ptr = [psb(4, [8, 128], BF16), psb(7, [8, 128], BF16)]
            hl = list(range(h0, 10))

            def trq(e, hl=hl):
                ins = None
                for hh in hl:
                    ins = e.transpose(out=ptr[hh // 8].ap[:, hh % 8, :], in_=qb.ap[:, hh, :], identity=identb.ap)
                return ins
            P.op("pe", trq, reads=qb.res() + identb.res(), writes=ptr[0].res() + ptr[1].res())
            if lat:
                dve(lambda e, t=t: e.tensor_copy(out=qT.ap[:, :, t * 128:(t + 1) * 128], in_=ptr[0].ap), reads=ptr[0].res(), writes=qT.res())
            dve(lambda e, t=t: e.tensor_copy(out=kT.ap[:, :, t * 128:(t + 1) * 128], in_=ptr[1].ap[:, 0:2, :]), reads=ptr[1].res(), writes=kT.res())

        if stop_after == "attn_proj":
            return
        scale = 128.0 ** -0.5
        for qg in range(4):
            qs = slice(qg * 512, (qg + 1) * 512)
            for hh in range(8):
                kv = hh // 4
                for kt in range(NT):
                    pS = psb(kt % 4, [512])
                    mm_group(pS.ap, [(kT.ap[:, kv, kt * 128:(kt + 1) * 128], qT.ap[:, hh, qs])], reads=kT.res() + qT.res(), writes=pS.res())
                    act(PT.ap[:, kt, :], pS.ap, AF.Exp, reads=pS.res(), writes=PT.sub(kt), scale=scale)
                pO, pR = psb(4, [512]), psb(5, [512])
                for kt in range(NT):
                    def pv_(e, kt=kt, kv=kv):
                        e.matmul(pO.ap, lhsT=V.ap[:, kt, kv * 128:(kv + 1) * 128], rhs=PT.ap[:, kt, :], start=(kt == 0), stop=(kt == NT - 1))
                        return e.matmul(pR.ap, lhsT=onesb.ap, rhs=PT.ap[:, kt, :], start=(kt == 0), stop=(kt == NT - 1))
                    P.op("pe", pv_, reads=V.sub(kt) + PT.sub(kt) + onesb.res(), writes=pO.res() + pR.res())
                dve(lambda e, pR=pR: e.reciprocal(out=rinv.ap, in_=pR.ap), reads=pR.res(), writes=rinv.res())
                dve(lambda e, pO=pO, hh=hh: e.tensor_tensor(out=OT.ap[:, hh, :], in0=pO.ap, in1=rinv.ap, op=ALU.mult), reads=pO.res() + rinv.res(), writes=OT.sub(hh))
            for ti in range(4):
                t = qg * 4 + ti
                for nh in range(2):
                    py = psb(6 + nh, [512])
                    mm_group(py.ap, [(OT.ap[:, hh, ti * 128:(ti + 1) * 128], wo.ap[:, hh, nh * 512:(nh + 1) * 512]) for hh in range(8)],
                             reads=OT.res() + wo.res(), writes=py.res())
                    resid_add(t, nh, py, gbc, tmpY)

    phases = ["gmlp", "moe0", "attn_proj", "attn", "moe1", "all"]
    upto = phases.index(stop_after)
    if upto >= 0:
        gmlp_phase()
    if upto >= 1:
        moe_sparse_phase(0, list(range(NT)))
    if upto >= 2:
        attn_phase()
    if upto >= 4:
        moe_sparse_phase(1, list(range(16)))

    nout = NT if dbg else 16
    for t in range(nout):
        dma("sp", out_d[t * 128:(t + 1) * 128, :], X.ap[:, t, :], reads=X.sub(t), writes=[("out", t)], key="out")
    P.op("sp", None, reads=[("out", t) for t in range(nout)])

    counters = P.finalize()
    sems = {k: es.enter_context(nc.semaphore("s%d" % i)) for i, k in enumerate(counters.keys())}
    with nc.Block() as block:
        P.emit(block, sems)
    es.close()
    return nc, P, counters


def _rope_tables():
    rows = 2048 // 64
    row = np.repeat(np.arange(rows, dtype=np.int32), 64).astype(np.float32)
    col = np.tile(np.arange(64, dtype=np.int32), rows).astype(np.float32)
    inv_freq = (1.0 / (np.float32(10000.0) ** (np.arange(0, 64, 2, dtype=np.float32) / np.float32(64)))).astype(np.float32)
    ang = np.stack([row[:, None] * inv_freq, col[:, None] * inv_freq], axis=1)
    return (np.cos(ang).astype(np.float32).reshape(2048, 64), np.sin(ang).astype(np.float32).reshape(2048, 64))


_CACHE = {}


def make_in_maps(inputs):
    f = lambda a: np.ascontiguousarray(np.asarray(a, dtype=np.float32))
    cos, sin = _rope_tables()
    shared = {
        "ada_w": f(inputs["ada_w"]),
        "ada_b": f(inputs["ada_b"]).reshape(96, 128),
        "norms": f(np.concatenate([inputs["norm_mix"], inputs["norm_ffn"]], axis=0)).reshape(32, 128),
        "gm_w_in": f(inputs["gm_w_in"][0]),
        "gm_b_in": f(inputs["gm_b_in"][0]).reshape(32, 128),
        "gm_v_gain": f(inputs["gm_v_gain"][0]).reshape(16, 128),
        "gm_w_s": f(inputs["gm_w_s"][0]),
        "gm_b_s": f(inputs["gm_b_s"][0]).reshape(1, 1024),
        "gm_w_out": f(inputs["gm_w_out"][0]),
        "at_w_qkv": f(inputs["at_w_qkv"][0]),
        "at_qk_gain": f(np.concatenate([inputs["at_q_gain"][0], inputs["at_k_gain"][0]])).reshape(1, 256),
        "at_w_o": f(inputs["at_w_o"][0]),
        "router_w": f(inputs["moe_router_w"]),
        "router_b": f(inputs["moe_router_b"]).reshape(2, 1, NEXP),
        "WG": np.ascontiguousarray(f(inputs["moe_w_gu"])[:, :, :, 0:1024].reshape(2, NEXP, 8, 128, 1024).transpose(0, 1, 3, 2, 4)).reshape(2 * NEXP * 128, 8192),
        "WU": np.ascontiguousarray(f(inputs["moe_w_gu"])[:, :, :, 1024:2048].reshape(2, NEXP, 8, 128, 1024).transpose(0, 1, 3, 2, 4)).reshape(2 * NEXP * 128, 8192),
        "WD": np.ascontiguousarray(f(inputs["moe_w_down"]).reshape(2, NEXP, 8, 128, 1024).transpose(0, 1, 3, 2, 4)).reshape(2 * NEXP * 128, 8192),
        "BGL": np.ascontiguousarray(f(inputs["moe_b_gu"]).reshape(2, NEXP, 16, 128).transpose(0, 1, 3, 2)).reshape(2 * NEXP * 128, 16),
        "cU0": np.triu(np.ones((128, 128), np.float32), 0),
        "cU1": np.triu(np.ones((128, 128), np.float32), 1),
        "cIota": np.ascontiguousarray(np.broadcast_to(np.arange(128, dtype=np.float32)[None, :], (128, 128))),
        "cPidx": np.ascontiguousarray(np.stack([np.arange(128, dtype=np.float32), np.zeros(128, np.float32)], axis=1)),
        "b_down": f(inputs["moe_b_down"]),
        "ident": np.eye(128, dtype=np.float32),
        "rope_cos": cos,
        "rope_sin": sin,
    }
    x, c, ctx, c_ctx = f(inputs["x"]), f(inputs["c"]), f(inputs["ctx"]), f(inputs["c_ctx"])
    maps = []
    for b in range(8):
        cvec = np.stack([c[b].reshape(8, 128).T, c_ctx.reshape(8, 128).T], axis=-1).reshape(128, 16)
        m = dict(shared)
        m["x"] = x[b]
        m["ctx"] = ctx[b]
        m["cvec"] = np.ascontiguousarray(cvec)
        maps.append(m)
    return maps


def kernel(**inputs):
    if "nc" not in _CACHE:
        _CACHE["nc"] = build_program("all")[0]
    nc = _CACHE["nc"]
    maps = make_in_maps(inputs)
    res = run_bass_kernel_spmd(nc, maps, core_ids=list(range(8)))
    return np.stack([r["out"] for r in res.results], axis=0).astype(np.float32)
```
